# Optimizing a Trainium2 kernel written in Bass

```python
import math
import jax, jax.numpy as jnp
from jax import lax
import numpy as np

D_MODEL = 4096
BATCH = 1
SEQ = 8192
DEPTH = 1

MIX_WIDTH = D_MODEL
MOBA_HEADS = 16
MOBA_HEAD_DIM = (MIX_WIDTH // 2) // MOBA_HEADS
MOBA_WIDTH = MOBA_HEADS * MOBA_HEAD_DIM
MOBA_BLOCK = 256
MOBA_TOPK = 3
MOBA_Q_CHUNK = 32
RET_HEADS = 8
RET_HEAD_DIM = (MIX_WIDTH - MOBA_WIDTH) // RET_HEADS
RET_WIDTH = RET_HEADS * RET_HEAD_DIM
RET_CHUNK = 512
ROPE_BASE = 10000.0
REL_BUCKETS = 32
REL_MAX_DIST = 128
IN_WIDTH = 3 * MOBA_WIDTH + 4 * RET_WIDTH
PEER_HEADS = 8
PEER_NKEYS = 128
PEER_EXPERTS = PEER_NKEYS * PEER_NKEYS
PEER_QDIM = 256
PEER_TOPK = 16
PEER_T_CHUNK = 64
NORM_EPS = 1e-6
GN_EPS = 1e-6
NEG = -1e30

kernel_name = 'hymba_moba_retnet_peer_layer'


def rmsnorm(x, g):
    xf = x.astype(jnp.float32)
    y = xf * lax.rsqrt(jnp.mean(xf * xf, axis=-1, keepdims=True) + NORM_EPS) * g.astype(jnp.float32)
    return y.astype(x.dtype)


def t5_bucket(n):
    n = jnp.maximum(n, 0)
    max_exact = REL_BUCKETS // 2
    nf = jnp.maximum(n, 1).astype(jnp.float32)
    large = max_exact + (jnp.log(nf / max_exact) / math.log(REL_MAX_DIST / max_exact)
                         * (REL_BUCKETS - max_exact)).astype(jnp.int32)
    large = jnp.minimum(large, REL_BUCKETS - 1)
    return jnp.where(n < max_exact, n, large)


def rotary(x, pos):
    half = x.shape[-1] // 2
    inv = ROPE_BASE ** (-jnp.arange(half, dtype=jnp.float32) / half)
    ang = pos.astype(jnp.float32)[:, None] * inv[None, :]
    cos = jnp.cos(ang)[None, :, None, :]
    sin = jnp.sin(ang)[None, :, None, :]
    xf = x.astype(jnp.float32)
    x1, x2 = xf[..., :half], xf[..., half:]
    return jnp.concatenate([x1 * cos - x2 * sin, x1 * sin + x2 * cos], axis=-1).astype(x.dtype)


def moba_attention(q, k, v, rel_bias):
    B, S, H, Dh = q.shape
    nblk = -(-S // MOBA_BLOCK)
    n_sel = min(MOBA_TOPK, nblk)
    Lp = nblk * MOBA_BLOCK
    pad = ((0, 0), (0, Lp - S), (0, 0), (0, 0))
    q, k, v = jnp.pad(q, pad), jnp.pad(k, pad), jnp.pad(v, pad)
    kb = k.reshape(B, nblk, MOBA_BLOCK, H, Dh)
    vb = v.reshape(B, nblk, MOBA_BLOCK, H, Dh)
    kmean = jnp.mean(kb.astype(jnp.float32), axis=2)
    kbh = kb.transpose(0, 3, 1, 2, 4)
    vbh = vb.transpose(0, 3, 1, 2, 4)
    scale = Dh ** -0.5
    n_chunks = Lp // MOBA_Q_CHUNK
    qc = q.reshape(B, n_chunks, MOBA_Q_CHUNK, H, Dh).transpose(1, 0, 2, 3, 4)
    blk_ids = jnp.arange(nblk)
    offs = jnp.arange(MOBA_BLOCK)
    bidx = jnp.arange(B)[:, None, None, None]
    hidx = jnp.arange(H)[None, None, :, None]

    def one_chunk(args):
        qi, ci = args
        start = ci * MOBA_Q_CHUNK
        cur = start // MOBA_BLOCK
        qpos = start + jnp.arange(MOBA_Q_CHUNK)
        qf = qi.astype(jnp.float32)
        gate = jnp.einsum('bqhd,bnhd->bqhn', qf, kmean)
        gate = jnp.where(blk_ids < cur, gate, NEG)
        _, sel = lax.top_k(gate, n_sel)
        sel_valid = sel < cur
        k_sel = kbh[bidx, hidx, sel]
        v_sel = vbh[bidx, hidx, sel]
        s_sel = jnp.einsum('bqhd,bqhjkd->bqhjk', qf * scale, k_sel.astype(jnp.float32))
        rel_sel = qpos[None, :, None, None, None] - (sel[..., None] * MOBA_BLOCK + offs)
        s_sel = s_sel + rel_bias[t5_bucket(rel_sel), hidx[..., None]].astype(jnp.float32)
        s_sel = jnp.where(sel_valid[..., None], s_sel, NEG)
        s_sel = s_sel.reshape(B, MOBA_Q_CHUNK, H, n_sel * MOBA_BLOCK)
        k_own = lax.dynamic_index_in_dim(kbh, cur, axis=2, keepdims=False)
        v_own = lax.dynamic_index_in_dim(vbh, cur, axis=2, keepdims=False)
        s_own = jnp.einsum('bqhd,bhkd->bqhk', qf * scale, k_own.astype(jnp.float32))
        rel_own = qpos[:, None] - (cur * MOBA_BLOCK + offs)[None, :]
        s_own = s_own + rel_bias[t5_bucket(rel_own)].transpose(0, 2, 1)[None].astype(jnp.float32)
        s_own = jnp.where((rel_own >= 0)[None, :, None, :], s_own, NEG)
        p = jax.nn.softmax(jnp.concatenate([s_sel, s_own], axis=-1), axis=-1)
        p_sel = p[..., :n_sel * MOBA_BLOCK].reshape(B, MOBA_Q_CHUNK, H, n_sel, MOBA_BLOCK)
        p_own = p[..., n_sel * MOBA_BLOCK:]
        out = (jnp.einsum('bqhjk,bqhjkd->bqhd', p_sel, v_sel.astype(jnp.float32))
               + jnp.einsum('bqhk,bhkd->bqhd', p_own, v_own.astype(jnp.float32)))
        return out.astype(q.dtype)

    out = lax.map(one_chunk, (qc, jnp.arange(n_chunks)))
    return out.transpose(1, 0, 2, 3, 4).reshape(B, Lp, H, Dh)[:, :S]


def retention(q, k, v):
    B, S, H, D = q.shape
    C = RET_CHUNK
    n = -(-S // C)
    Lp = n * C
    pad = ((0, 0), (0, Lp - S), (0, 0), (0, 0))
    q, k, v = (jnp.pad(t.astype(jnp.float32), pad) for t in (q, k, v))
    log_g = jnp.log1p(-(2.0 ** (-5.0 - jnp.arange(H, dtype=jnp.float32))))
    idx = jnp.arange(C, dtype=jnp.float32)
    diff = idx[:, None] - idx[None, :]
    decay_intra = jnp.where(diff >= 0, jnp.exp(log_g[:, None, None] * jnp.maximum(diff, 0.0)), 0.0)
    q_decay = jnp.exp(log_g[:, None] * (idx + 1.0))[None, :, :, None]
    k_decay = jnp.exp(log_g[:, None] * (C - 1.0 - idx))[None, :, :, None]
    chunk_decay = jnp.exp(log_g * C)[None, :, None, None]
    to_chunks = lambda t: t.reshape(B, n, C, H, D).transpose(1, 0, 3, 2, 4)

    def step(state, inp):
        qc, kc, vc = inp
        inner = jnp.einsum('bhid,bhjd->bhij', qc, kc) * decay_intra[None]
        o = (jnp.einsum('bhij,bhjd->bhid', inner, vc)
             + jnp.einsum('bhid,bhde->bhie', qc * q_decay, state))
        state = state * chunk_decay + jnp.einsum('bhjd,bhje->bhde', kc * k_decay, vc)
        return state, o

    state0 = jnp.zeros((B, H, D, D), jnp.float32)
    _, o = lax.scan(step, state0, (to_chunks(q), to_chunks(k), to_chunks(v)))
    return o.transpose(1, 0, 3, 2, 4).reshape(B, Lp, H, D)[:, :S]


def head_group_norm(o):
    mu = jnp.mean(o, axis=-1, keepdims=True)
    var = jnp.mean(jnp.square(o - mu), axis=-1, keepdims=True)
    return (o - mu) * lax.rsqrt(var + GN_EPS)


def peer_ffn(xn, w_q, sub_keys, u, v):
    B, S, Dm = xn.shape
    T = B * S
    tok = xn.reshape(T, Dm)
    q = (tok @ w_q).astype(jnp.float32).reshape(T, PEER_HEADS, 2, PEER_QDIM // 2)
    s = jnp.einsum('thcd,hcnd->thcn', q, sub_keys.astype(jnp.float32))
    sv, si = lax.top_k(s, PEER_TOPK)
    cand_s = (sv[..., 0, :, None] + sv[..., 1, None, :]).reshape(T, PEER_HEADS, PEER_TOPK * PEER_TOPK)
    cand_e = (si[..., 0, :, None] * PEER_NKEYS + si[..., 1, None, :]).reshape(T, PEER_HEADS, PEER_TOPK * PEER_TOPK)
    top_s, top_pos = lax.top_k(cand_s, PEER_TOPK)
    experts = jnp.take_along_axis(cand_e, top_pos, axis=-1)
    gates = jax.nn.softmax(top_s, axis=-1)
    n_chunks = -(-T // PEER_T_CHUNK)
    Tp = n_chunks * PEER_T_CHUNK
    tok_p = jnp.pad(tok, ((0, Tp - T), (0, 0))).reshape(n_chunks, PEER_T_CHUNK, Dm)
    exp_p = jnp.pad(experts, ((0, Tp - T), (0, 0), (0, 0))).reshape(n_chunks, PEER_T_CHUNK, PEER_HEADS, PEER_TOPK)
    gate_p = jnp.pad(gates, ((0, Tp - T), (0, 0), (0, 0))).reshape(n_chunks, PEER_T_CHUNK, PEER_HEADS, PEER_TOPK)

    def one_chunk(args):
        tc, ec, gc = args
        u_sel = u[ec]
        act = jax.nn.gelu(jnp.einsum('td,thkd->thk', tc, u_sel).astype(jnp.float32), approximate=False)
        w = (act * gc).astype(v.dtype)
        return jnp.einsum('thk,thkd->td', w, v[ec])

    out = lax.map(one_chunk, (tok_p, exp_p, gate_p))
    return out.reshape(Tp, Dm)[:T].reshape(B, S, Dm).astype(xn.dtype)


def setup_inputs(seed: int = 0) -> dict:
    key = jax.random.key(seed)
    ks = jax.random.split(key, 12)
    f32 = jnp.float32
    x = jax.random.normal(ks[0], (BATCH, SEQ, D_MODEL), f32)
    norm_mix_g = 1.0 + 0.02 * jax.random.normal(ks[1], (DEPTH, D_MODEL), f32)
    w_in = jax.random.normal(ks[2], (DEPTH, D_MODEL, IN_WIDTH), f32) * D_MODEL ** -0.5
    w_out = jax.random.normal(ks[3], (DEPTH, MIX_WIDTH, D_MODEL), f32) * MIX_WIDTH ** -0.5
    rel_bias = 0.5 * jax.random.normal(ks[4], (REL_BUCKETS, MOBA_HEADS), f32)
    norm_ffn_g = 1.0 + 0.02 * jax.random.normal(ks[5], (DEPTH, D_MODEL), f32)
    peer_w_q = jax.random.normal(ks[6], (DEPTH, D_MODEL, PEER_HEADS * PEER_QDIM), f32) * D_MODEL ** -0.5
    peer_sub_keys = jax.random.normal(ks[7], (DEPTH, PEER_HEADS, 2, PEER_NKEYS, PEER_QDIM // 2), f32) * (PEER_QDIM // 2) ** -0.5
    peer_u = jax.random.normal(ks[8], (DEPTH, PEER_EXPERTS, D_MODEL), f32) * D_MODEL ** -0.5
    peer_v = jax.random.normal(ks[9], (DEPTH, PEER_EXPERTS, D_MODEL), f32) * PEER_HEADS ** -0.5
    norm_final_g = 1.0 + 0.02 * jax.random.normal(ks[10], (D_MODEL,), f32)
    return {'x': x, 'norm_mix_g': norm_mix_g, 'w_in': w_in, 'w_out': w_out, 'rel_bias': rel_bias,
            'norm_ffn_g': norm_ffn_g, 'peer_w_q': peer_w_q, 'peer_sub_keys': peer_sub_keys,
            'peer_u': peer_u, 'peer_v': peer_v, 'norm_final_g': norm_final_g}


def reference(x, norm_mix_g, w_in, w_out, rel_bias, norm_ffn_g, peer_w_q, peer_sub_keys,
              peer_u, peer_v, norm_final_g):
    B, S, _ = x.shape
    pos = jnp.arange(S, dtype=jnp.int32)
    splits = [MOBA_WIDTH, 2 * MOBA_WIDTH, 3 * MOBA_WIDTH, 3 * MOBA_WIDTH + RET_WIDTH,
              3 * MOBA_WIDTH + 2 * RET_WIDTH, 3 * MOBA_WIDTH + 3 * RET_WIDTH]
    h = x
    for layer in range(DEPTH):
        n = rmsnorm(h, norm_mix_g[layer])
        proj = n @ w_in[layer]
        mq, mk, mv, rq, rk, rv, rg = jnp.split(proj, splits, axis=-1)
        mshape = (B, S, MOBA_HEADS, MOBA_HEAD_DIM)
        a_out = moba_attention(mq.reshape(mshape), mk.reshape(mshape), mv.reshape(mshape), rel_bias)
        rshape = (B, S, RET_HEADS, RET_HEAD_DIM)
        rq = rotary(rq.reshape(rshape), pos)
        rk = rotary(rk.reshape(rshape), pos) * (RET_HEAD_DIM ** -0.5)
        r_out = head_group_norm(retention(rq, rk, rv.reshape(rshape))).reshape(B, S, RET_WIDTH)
        r_out = jax.nn.silu(rg.astype(jnp.float32)) * r_out
        mix = jnp.concatenate([a_out.reshape(B, S, MOBA_WIDTH).astype(h.dtype), r_out.astype(h.dtype)], axis=-1)
        h = h + mix @ w_out[layer]
        h = h + peer_ffn(rmsnorm(h, norm_ffn_g[layer]), peer_w_q[layer], peer_sub_keys[layer],
                         peer_u[layer], peer_v[layer])
    return rmsnorm(h, norm_final_g)
```

```python
import math
from contextlib import ExitStack
import numpy as np
import ml_dtypes
import concourse.bass as bass
import concourse.mybir as mybir
from concourse.bass_utils import run_bass_kernel_spmd

F32 = mybir.dt.float32
BF16 = mybir.dt.bfloat16
U32 = mybir.dt.uint32
I32 = mybir.dt.int32
AF = mybir.ActivationFunctionType
ALU = mybir.AluOpType
AX = mybir.AxisListType

ENGINES = ("tensor", "vector", "scalar", "gpsimd", "sync")
COMPUTE = ("tensor", "vector", "scalar", "gpsimd")
S = 8192
D = 4096
NCORE = 8
TC = 1024
NEGM = -30000.0
DEBUG = False


class Buf:
    __slots__ = ("name", "last_write", "readers")

    def __init__(self, name):
        self.name = name
        self.last_write = None
        self.readers = []


def _compact(readers):
    best = {}
    for sem, val, src in readers:
        k = id(sem)
        if k not in best or best[k][1] < val:
            best[k] = (sem, val, src)
    return list(best.values())


class Prog:
    def __init__(self, nc, stack):
        self.nc = nc
        self.q = {e: [] for e in ENGINES}
        self.cnt = {e: 0 for e in COMPUTE}
        self.known = {e: {} for e in ENGINES}
        self.fin = {}
        self._stack = stack
        self.esem = {e: stack.enter_context(nc.semaphore("cnt_" + e)) for e in COMPUTE}
        self.nsem = 0

    def new_sem(self, name=None):
        self.nsem += 1
        s = self._stack.enter_context(self.nc.semaphore("s%d_%s" % (self.nsem, name or "")))
        return [s, 0]

    def _need(self, eng, deps):
        out = {}
        kn = self.known[eng]
        for d in deps:
            if d is None:
                continue
            sem, val, src = d
            if src == "tensor" and eng == "tensor":
                continue
            k = id(sem)
            if kn.get(k, 0) >= val:
                continue
            if k not in out or out[k][1] < val:
                out[k] = (sem, val)
        for k, (sem, val) in out.items():
            kn[k] = val
        return list(out.values())

    def _deps(self, reads, writes):
        deps = []
        for b in reads:
            deps.append(b.last_write)
        for b in writes:
            deps.append(b.last_write)
            deps.extend(b.readers)
        return deps

    def _mark(self, tok, reads, writes):
        for b in reads:
            b.readers.append(tok)
            if len(b.readers) > 6:
                b.readers = _compact(b.readers)
        for b in writes:
            b.last_write = tok
            b.readers = []

    def op(self, eng, fn, reads=(), writes=()):
        waits = self._need(eng, self._deps(reads, writes))
        self.cnt[eng] += 1
        sem = self.esem[eng]
        self.q[eng].append((fn, waits, (sem, 1)))
        tok = (sem, self.cnt[eng], eng)
        self._mark(tok, reads, writes)
        return tok

    def dma(self, eng, fn, reads=(), writes=(), semc=None, inc=16):
        if semc is None:
            semc = self.new_sem("d")
        waits = self._need(eng, self._deps(reads, writes))
        semc[1] += inc
        sem, val = semc[0], semc[1]
        self.q[eng].append((fn, waits, (sem, inc)))
        tok = (sem, val, None)
        self._mark(tok, reads, writes)
        k = id(sem)
        if k not in self.fin or self.fin[k][1] < val:
            self.fin[k] = (sem, val)
        return tok

    def barrier(self):
        allw = [(s, v, None) for (s, v) in self.fin.values()]
        for e in COMPUTE:
            if self.cnt[e] > 0:
                allw.append((self.esem[e], self.cnt[e], None))
        for e in ENGINES:
            w = self._need(e, allw)
            if w:
                self.q[e].append((None, w, None))

    def replay(self):
        self.barrier()
        with self.nc.Block() as block:
            for e in ENGINES:
                items = self.q[e]

                def body(engine, items=items):
                    for fn, waits, inc in items:
                        for sem, val in waits:
                            engine.wait_ge(sem, val)
                        if fn is not None:
                            ins = fn(engine)
                            if inc is not None:
                                ins.then_inc(inc[0], inc[1])

                getattr(block, e)(body)
        self.q = {e: [] for e in ENGINES}


def build_nc():
    nc = bass.Bass("TRN2", target_bir_lowering=False)

    def din(name, shape, dt=F32):
        return nc.dram_tensor(name, list(shape), dt, kind="ExternalInput").ap()

    def dscr(name, shape, dt):
        return nc.dram_tensor(name, list(shape), dt).ap()

    xT = din("xT", [D, S]); xTc = din("xTc", [D, TC])
    g1 = din("g1", [128, 32]); g2 = din("g2", [128, 32]); gf = din("gf", [128, 32])
    wfm = din("wfm", [128, 32, 1280]); wtm = din("wtm", [128, 32, 512])
    cosT = din("cosT", [128, S]); sinT = din("sinT", [128, S])
    kdec = din("kdec", [128, 64]); DTd = din("DT", [128, 4, 512]); QDd = din("QD", [128, 512]); CDd = din("CD", [128, 1])
    rbc = din("rbc", [2, 32])
    CBd = din("CB", [128, 64, 32]); PASTd = din("PAST", [128, 64, 32]); OWNd = din("OWN", [128, 64, 32])
    Ealld = din("Eall", [32, 32, 128], BF16); OHd = din("OH", [128, 31, 1024], BF16); CAUSd = din("CAUSW", [128, 1024])
    identd = din("identf", [128, 128])
    wod = din("wo", [32, 128, 32, 128]); gidx = din("gidx", [128, 32], I32)
    wqd = din("wq", [16, 128, 32, 128]); keyTd = din("keyT", [128, 16, 128])
    UTd = din("UT", [128, 128, 32 * 128]); Vd = din("V", [16384, D])
    iotad = din("iota128", [128, 128])
    YT = nc.dram_tensor("YT", [D, TC], F32, kind="ExternalOutput").ap()

    MQT = dscr("MQT", [2, 128, S], BF16); MKT = dscr("MKT", [2, 128, S], BF16); MV = dscr("MV", [S, 256], BF16)
    RQT = dscr("RQT", [2, 128, S], BF16); RKT = dscr("RKT", [2, 128, S], BF16); RGT = dscr("RGT", [2, 128, S], F32)
    RKd = dscr("RKd", [S, 256], BF16); RV = dscr("RV", [S, 256], BF16)
    AGIN_t = nc.dram_tensor("AGIN", [512 * 8, 1024], BF16)
    AGOUT_t = nc.dram_tensor("AGOUT", [D * 8, 1024], BF16)
    AGIN = AGIN_t.ap(); AGOUT = AGOUT_t.ap()
    if DEBUG:
        H2T = nc.dram_tensor("H2T", [D, TC], F32, kind="ExternalOutput").ap()
        DBGM = nc.dram_tensor("DBGM", [128, 32, TC], BF16, kind="ExternalOutput").ap()
    else:
        H2T = dscr("H2T", [D, TC], F32)
    XN2T = dscr("XN2T", [D, TC], BF16); H3T = dscr("H3T", [D, TC], F32)

    SC = 128 ** -0.5

    with ExitStack() as top:
        P = Prog(nc, top)
        psA = top.enter_context(nc.psum_tensor("psA", [128, 2048], F32))
        psB = top.enter_context(nc.psum_tensor("psB", [128, 2048], F32))
        ps = [psA[:, i * 512:(i + 1) * 512] for i in range(4)] + [psB[:, i * 512:(i + 1) * 512] for i in range(4)]
        Bps = [Buf("ps%d" % i) for i in range(8)]
        dB = {n: Buf(n) for n in "MQT MKT MV RQT RKT RGT RKd RV AGIN AGOUT H2T XN2T H3T".split()}

        def OP(eng, reads, writes, f):
            return P.op(eng, f, reads=reads, writes=writes)

        with ExitStack() as ph:
            def sb(name, shape, dt):
                return ph.enter_context(nc.sbuf_tensor("a1_" + name, list(shape), dt))
            wfm_sb = sb("wfm", [128, 32, 1280], BF16); wtm_sb = sb("wtm", [128, 32, 512], BF16)
            Bw = Buf("w")
            wst = [sb("wst%d" % i, [128, 1280], F32) for i in range(2)]; Bwst = [Buf("wst0"), Buf("wst1")]
            swst = [P.new_sem("wst0"), P.new_sem("wst1")]
            g1_sb = sb("g1", [128, 32], F32); Bg1 = Buf("g1")
            kdec_sb = sb("kdec", [128, 64], F32)
            ones_b = sb("ones", [128, 128], BF16); Bones = Buf("ones")
            ident = sb("ident", [128, 128], F32); Bid = Buf("ident")
            P.dma("sync", lambda e: e.dma_start(out=g1_sb[:], in_=g1[:, :]), writes=[Bg1], semc=None)
            P.dma("sync", lambda e: e.dma_start(out=kdec_sb[:], in_=kdec[:, :]), writes=[Bg1], semc=None)
            P.dma("sync", lambda e: e.dma_start(out=ident[:], in_=identd[:, :]), writes=[Bid], semc=None)
            OP("vector", [], [Bones], lambda e: e.memset(ones_b[:], 1.0))
            n = 0
            for (src, dst, width) in ((wfm, wfm_sb, 1280), (wtm, wtm_sb, 512)):
                for kt in range(32):
                    s = n % 2; n += 1
                    P.dma("sync", lambda e, s=s, kt=kt, src=src, width=width: e.dma_start(out=wst[s][:, 0:width], in_=src[:, kt, :]),
                          writes=[Bwst[s]], semc=swst[s])
                    OP("vector", [Bwst[s], Bg1], [Bw],
                       lambda e, s=s, kt=kt, dst=dst, width=width: e.tensor_scalar(dst[:, kt, :], wst[s][:, 0:width], g1_sb[:, kt:kt + 1], None, ALU.mult))
            xb = [sb("xb%d" % i, [128, 32, 256], BF16) for i in range(2)]; Bxb = [Buf("xb0"), Buf("xb1")]
            sxb = [P.new_sem("xb0"), P.new_sem("xb1")]
            xsq = sb("xsq", [128, 32, 256], BF16); Bxsq = Buf("xsq")
            cst = [sb("cst%d" % i, [128, 2, 256], F32) for i in range(2)]; Bcst = [Buf("c0"), Buf("c1")]
            sc_ = [P.new_sem("cs0"), P.new_sem("cs1")]
            rsA = sb("rsA", [128, 256], F32); rsB = sb("rsB", [128, 256], F32); rstd = sb("rstd", [128, 256], F32)
            Brs = Buf("rs"); Brstd = Buf("rstd")
            rstt = sb("rstt", [128, 2], F32); Brstt = Buf("rstt")
            qk_st = [sb("qk%d" % i, [128, 8, 256], BF16) for i in range(2)]; Bqk = [Buf("qk0"), Buf("qk1")]
            g_st = [sb("gs%d" % i, [128, 2, 256], F32) for i in range(2)]; Bgs = [Buf("gs0"), Buf("gs1")]
            rot = sb("rot", [128, 4, 256], F32); Brot = Buf("rot")
            tmp = sb("tmp", [128, 4, 256], F32); Btmp = Buf("tmp")
            tm_st = [[sb("tm%d_%d" % (i, j), [128, 3, 256], BF16) for j in range(2)] for i in range(2)]
            Btm = [[Buf("tm"), Buf("tm")], [Buf("tm"), Buf("tm")]]
            identb = sb("identb", [128, 128], BF16)
            OP("vector", [Bid], [Bid], lambda e: e.tensor_copy(identb[:], ident[:]))
            psb6 = ps[6][:].bitcast(BF16)
            stm_ = [[P.new_sem("tm"), P.new_sem("tm")], [P.new_sem("tm"), P.new_sem("tm")]]
            sqk_ = [P.new_sem("qk0"), P.new_sem("qk1")]; sgs_ = [P.new_sem("gs0"), P.new_sem("gs1")]
            xTv = xT.rearrange("(k p) t -> p k t", p=128)
            MQTv = MQT.rearrange("h p t -> p h t"); MKTv = MKT.rearrange("h p t -> p h t")
            RQTv = RQT.rearrange("h p t -> p h t"); RKTv = RKT.rearrange("h p t -> p h t"); RGTv = RGT.rearrange("h p t -> p h t")
            NCH = S // 256
            for tc in range(NCH):
                s = tc % 2
                t0 = tc * 256
                P.dma("gpsimd", lambda e, s=s, t0=t0: e.dma_start(out=xb[s][:], in_=xTv[:, :, t0:t0 + 256]), writes=[Bxb[s]], semc=sxb[s])
                P.dma("sync", lambda e, s=s, t0=t0: e.dma_start(out=cst[s][:, 0, :], in_=cosT[:, t0:t0 + 256]), writes=[Bcst[s]], semc=sc_[s])
                P.dma("sync", lambda e, s=s, t0=t0: e.dma_start(out=cst[s][:, 1, :], in_=sinT[:, t0:t0 + 256]), writes=[Bcst[s]], semc=sc_[s])
                OP("scalar", [Bxb[s]], [Bxsq], lambda e, s=s: e.activation(xsq[:], xb[s][:], AF.Square))
                for kt in range(32):
                    OP("tensor", [Bxsq, Bones], [Bps[0]], lambda e, kt=kt: e.matmul(ps[0][:, 0:256], ones_b[:], xsq[:, kt, :], start=(kt == 0), stop=(kt == 31)))
                OP("vector", [Bps[0]], [Brs], lambda e: e.tensor_scalar(rsA[:], ps[0][:, 0:256], 1.0 / D, 1e-6, ALU.mult, ALU.add))
                OP("scalar", [Brs], [Brs], lambda e: e.activation(rsB[:], rsA[:], AF.Sqrt))
                OP("vector", [Brs], [Brstd], lambda e: e.reciprocal(rstd[:], rsB[:]))
                for tt in range(2):
                    OP("tensor", [Brstd, Bid], [Bps[1]], lambda e, tt=tt: e.transpose(ps[1][:, tt * 128:(tt + 1) * 128], rstd[:, tt * 128:(tt + 1) * 128], ident[:]))
                OP("vector", [Bps[1]], [Brstt], lambda e: e.tensor_copy(rstt[:], ps[1][:, 0:256].rearrange("p (a b) -> p a b", b=128)[:, :, 0]))
                for ct in range(10):
                    bk = 2 + ct % 3
                    for kt in range(32):
                        OP("tensor", [Bxb[s], Bw], [Bps[bk]], lambda e, kt=kt, ct=ct, bk=bk, s=s: e.matmul(ps[bk][:, 0:256], wfm_sb[:, kt, ct * 128:(ct + 1) * 128], xb[s][:, kt, :], start=(kt == 0), stop=(kt == 31)))
                    if ct < 4:
                        OP("vector", [Bps[bk], Brstd], [Bqk[s]], lambda e, ct=ct, bk=bk, s=s: e.tensor_tensor(qk_st[s][:, ct, :], ps[bk][:, 0:256], rstd[:], ALU.mult))
                    elif ct < 6:
                        OP("vector", [Bps[bk], Brstd], [Brot], lambda e, ct=ct, bk=bk: e.tensor_tensor(rot[:, ct - 4, :], ps[bk][:, 0:256], rstd[:], ALU.mult))
                    elif ct < 8:
                        OP("vector", [Bps[bk], Brstd], [Brot], lambda e, ct=ct, bk=bk: e.scalar_tensor_tensor(rot[:, ct - 4, :], ps[bk][:, 0:256], 0.0625, rstd[:], ALU.mult, ALU.mult))
                    else:
                        OP("vector", [Bps[bk], Brstd], [Bgs[s]], lambda e, ct=ct, bk=bk, s=s: e.tensor_tensor(g_st[s][:, ct - 8, :], ps[bk][:, 0:256], rstd[:], ALU.mult))
                for a, dst in ((0, 4), (2, 6)):
                    OP("vector", [Brot, Bcst[s]], [Btmp], lambda e, a=a, s=s: e.tensor_tensor(tmp[:, 0, :], rot[:, a, :], cst[s][:, 0, :], ALU.mult))
                    OP("vector", [Brot, Bcst[s]], [Btmp], lambda e, a=a, s=s: e.tensor_tensor(tmp[:, 1, :], rot[:, a + 1, :], cst[s][:, 1, :], ALU.mult))
                    OP("vector", [Brot, Bcst[s]], [Btmp], lambda e, a=a, s=s: e.tensor_tensor(tmp[:, 2, :], rot[:, a, :], cst[s][:, 1, :], ALU.mult))
                    OP("vector", [Brot, Bcst[s]], [Btmp], lambda e, a=a, s=s: e.tensor_tensor(tmp[:, 3, :], rot[:, a + 1, :], cst[s][:, 0, :], ALU.mult))
                    OP("vector", [Btmp], [Bqk[s]], lambda e, dst=dst, s=s: e.tensor_tensor(qk_st[s][:, dst, :], tmp[:, 0, :], tmp[:, 1, :], ALU.subtract))
                    OP("vector", [Btmp], [Bqk[s]], lambda e, dst=dst, s=s: e.tensor_tensor(qk_st[s][:, dst + 1, :], tmp[:, 2, :], tmp[:, 3, :], ALU.add))
                for tt in range(2):
                    gt = tc * 2 + tt
                    for kt in range(32):
                        OP("tensor", [Bxb[s], Bw], [Bps[5]], lambda e, kt=kt, tt=tt, s=s: e.matmul(ps[5][:, 0:512], xb[s][:, kt, tt * 128:(tt + 1) * 128], wtm_sb[:, kt, 0:512], start=(kt == 0), stop=(kt == 31)))
                    OP("scalar", [Bps[5], Brstt], [Btm[s][tt]], lambda e, tt=tt, s=s: e.activation(tm_st[s][tt][:, 0, :], ps[5][:, 0:256], AF.Copy, scale=rstt[:, tt:tt + 1]))
                    OP("scalar", [Bps[5], Brstt], [Btm[s][tt]], lambda e, tt=tt, s=s: e.activation(tm_st[s][tt][:, 2, :], ps[5][:, 256:512], AF.Copy, scale=rstt[:, tt:tt + 1]))
                    for hf in range(2):
                        OP("tensor", [Bqk[s], Bid], [Bps[6]], lambda e, tt=tt, s=s, hf=hf: e.transpose(psb6[:, hf * 128:(hf + 1) * 128], qk_st[s][:, 6 + hf, tt * 128:(tt + 1) * 128], identb[:]))
                    OP("vector", [Bps[6], Bg1], [Btm[s][tt]], lambda e, tt=tt, s=s, gt=gt: e.tensor_scalar(tm_st[s][tt][:, 1, :], psb6[:, 0:256], kdec_sb[:, gt:gt + 1], None, ALU.mult))
                    r0 = t0 + tt * 128
                    P.dma("sync", lambda e, tt=tt, s=s, r0=r0: e.dma_start(out=MV[r0:r0 + 128, :], in_=tm_st[s][tt][:, 0, :]), reads=[Btm[s][tt]], writes=[dB["MV"]], semc=stm_[s][tt])
                    P.dma("sync", lambda e, tt=tt, s=s, r0=r0: e.dma_start(out=RKd[r0:r0 + 128, :], in_=tm_st[s][tt][:, 1, :]), reads=[Btm[s][tt]], writes=[dB["RKd"]], semc=stm_[s][tt])
                    P.dma("sync", lambda e, tt=tt, s=s, r0=r0: e.dma_start(out=RV[r0:r0 + 128, :], in_=tm_st[s][tt][:, 2, :]), reads=[Btm[s][tt]], writes=[dB["RV"]], semc=stm_[s][tt])
                for (dv, lo, nm) in ((MQTv, 0, "MQT"), (MKTv, 2, "MKT"), (RQTv, 4, "RQT"), (RKTv, 6, "RKT")):
                    P.dma("sync", lambda e, dv=dv, lo=lo, s=s, t0=t0: e.dma_start(out=dv[:, :, t0:t0 + 256], in_=qk_st[s][:, lo:lo + 2, :]), reads=[Bqk[s]], writes=[dB[nm]], semc=sqk_[s])
                P.dma("sync", lambda e, s=s, t0=t0: e.dma_start(out=RGTv[:, :, t0:t0 + 256], in_=g_st[s][:]), reads=[Bgs[s]], writes=[dB["RGT"]], semc=sgs_[s])
            P.replay()

        AGINv = AGIN.rearrange("(f b) t -> f b t", b=8)

        with ExitStack() as ph:
            def sb(name, shape, dt):
                return ph.enter_context(nc.sbuf_tensor("a2_" + name, list(shape), dt))
            ones_b = sb("ones", [128, 128], BF16); identb = sb("identb", [128, 128], BF16); ident = sb("ident", [128, 128], F32)
            Bc = Buf("consts")
            CB = sb("CB", [128, 64, 32], F32); PAST = sb("PAST", [128, 64, 32], F32); OWN = sb("OWN", [128, 64, 32], F32)
            Eall = sb("Eall", [32, 32, 128], BF16); OH = sb("OH", [128, 31, 1024], BF16); CAUS = sb("CAUS", [128, 1024], F32)
            for (dst, src) in ((CB, CBd), (PAST, PASTd), (OWN, OWNd), (Eall, Ealld), (OH, OHd), (CAUS, CAUSd), (ident, identd)):
                P.dma("sync", lambda e, dst=dst, src=src: e.dma_start(out=dst[:], in_=src), writes=[Bc], semc=None)
            OP("vector", [], [Bc], lambda e: e.memset(ones_b[:], 1.0))
            OP("vector", [Bc], [Bc], lambda e: e.tensor_copy(identb[:], ident[:]))
            KT = sb("KT", [128, S], BF16); Vs = sb("V", [128, 64, 128], BF16); BKV = Buf("KV")
            rb = sb("rb", [128, 32], F32); rbrel = sb("rbrel", [128, 32], F32); c1 = sb("c1", [128, 1], F32); Brb = Buf("rb")
            Wf = sb("Wf", [128, 1024], F32); Wn = sb("Wn", [128, 1024], BF16); BW = Buf("W")
            kmf = sb("kmf", [128, 32], F32); kmT = sb("kmT", [128, 32], BF16); Bkm = Buf("km")
            QT = [sb("QT%d" % i, [128, 512], BF16) for i in range(2)]; BQ = [Buf("q0"), Buf("q1")]; sQ = [P.new_sem("q0"), P.new_sem("q1")]
            gm = sb("gm", [128, 4, 32], F32); m8 = sb("m8", [128, 4, 8], F32); sel = sb("sel", [128, 4, 32], F32); NM = sb("NM", [128, 4, 32], F32)
            Bgm = Buf("gm"); Bm8 = Buf("m8"); Bsel = Buf("sel"); BNM = Buf("NM")
            NMT = sb("NMT", [32, 512], BF16); BNMT = Buf("NMT")
            PT = [sb("PT%d" % i, [128, 512], BF16) for i in range(3)]; BPT = [Buf("pt0"), Buf("pt1"), Buf("pt2")]
            rinv = sb("rinv", [128, 512], F32); Brinv = Buf("rinv")
            acc = sb("acc", [128, 512], F32); Bacc = Buf("acc"); ones_f = sb("ones_f", [128, 128], F32)
            OP("vector", [], [Bc], lambda e: e.memset(ones_f[:], 1.0))
            ob = [sb("ob%d" % i, [128, 512], BF16) for i in range(2)]; Bob = [Buf("ob0"), Buf("ob1")]; sob = [P.new_sem("ob0"), P.new_sem("ob1")]
            pscnt = 0
            for hh in range(2):
                P.dma("sync", lambda e, hh=hh: e.dma_start(out=KT[:], in_=MKT[hh, :, :]), reads=[dB["MKT"]], writes=[BKV], semc=None)
                P.dma("sync", lambda e, hh=hh: e.dma_start(out=Vs[:], in_=MV[:, hh * 128:(hh + 1) * 128].rearrange("(k p) d -> p k d", p=128)), reads=[dB["MV"]], writes=[BKV], semc=None)
                P.dma("sync", lambda e, hh=hh: e.dma_start(out=rb[:], in_=rbc[hh:hh + 1, :].partition_broadcast(128)), writes=[Brb], semc=None)
                OP("vector", [Brb], [Brb], lambda e: e.tensor_scalar(rbrel[:], rb[:], rb[:, 31:32], 1.0 / SC, ALU.subtract, ALU.mult))
                OP("vector", [Brb], [Brb], lambda e: e.tensor_scalar(c1[:], rb[:, 31:32], 1.0 / SC, -NEGM, ALU.mult, ALU.add))
                OP("vector", [Bc], [BW], lambda e: e.tensor_copy(Wf[:], CAUS[:]))
                for b in range(31):
                    OP("vector", [Bc, Brb, BW], [BW], lambda e, b=b: e.scalar_tensor_tensor(Wf[:], OH[:, b, :], rbrel[:, b:b + 1], Wf[:], ALU.mult, ALU.add))
                OP("vector", [BW], [BW], lambda e: e.tensor_copy(Wn[:], Wf[:]))
                OP("vector", [BKV], [Bkm], lambda e: e.tensor_reduce(kmf[:], KT[:].rearrange("p (b k) -> p b k", k=256), AX.X, ALU.add))
                OP("vector", [Bkm], [Bkm], lambda e: e.tensor_scalar(kmT[:], kmf[:], 1.0 / 256, None, ALU.mult))
                for qc in range(16):
                    s = qc % 2
                    P.dma("sync", lambda e, hh=hh, qc=qc, s=s: e.dma_start(out=QT[s][:], in_=MQT[hh, :, qc * 512:(qc + 1) * 512]), reads=[dB["MQT"]], writes=[BQ[s]], semc=sQ[s])
                    for qt in range(4):
                        OP("tensor", [BQ[s], Bkm], [Bps[0]], lambda e, qt=qt, s=s: e.matmul(ps[0][:, qt * 32:(qt + 1) * 32], QT[s][:, qt * 128:(qt + 1) * 128], kmT[:], start=True, stop=True))
                    g0 = qc * 4
                    OP("vector", [Bps[0], Bc], [Bgm], lambda e, g0=g0: e.tensor_tensor(gm[:], ps[0][:, 0:128].rearrange("p (a b) -> p a b", b=32), CB[:, g0:g0 + 4, :], ALU.add))
                    for qt in range(4):
                        OP("vector", [Bgm], [Bm8], lambda e, qt=qt: e.max(m8[:, qt, :], gm[:, qt, :]))
                    for qt in range(4):
                        OP("vector", [Bgm, Bm8, Bc], [Bsel], lambda e, qt=qt, g0=g0: e.scalar_tensor_tensor(sel[:, qt, :], gm[:, qt, :], m8[:, qt, 2:3], PAST[:, g0 + qt, :], ALU.is_ge, ALU.mult))
                    OP("vector", [Bsel, Bc], [Bsel], lambda e, g0=g0: e.tensor_tensor(sel[:], sel[:], OWN[:, g0:g0 + 4, :], ALU.add))
                    OP("vector", [Bsel, Brb], [BNM], lambda e: e.tensor_scalar(NM[:], sel[:], c1[:, 0:1], NEGM, ALU.mult, ALU.add))
                    for qt in range(4):
                        OP("tensor", [BNM, Bc], [Bps[1]], lambda e, qt=qt: e.transpose(ps[1][0:32, qt * 128:(qt + 1) * 128], NM[:, qt, :], ident[:]))
                    OP("scalar", [Bps[1]], [BNMT], lambda e: e.activation(NMT[:], ps[1][0:32, :], AF.Copy))
                    nkt = 4 * qc + 4

                    def emit_S(kt, qc=qc, s=s):
                        bk = 2 + (kt % 2)
                        near = kt >= 4 * qc - 1
                        OP("tensor", [BKV, BQ[s]], [Bps[bk]], lambda e: e.matmul(ps[bk][:], KT[:, kt * 128:(kt + 1) * 128], QT[s][:], start=True, stop=False))
                        OP("tensor", [Bc, BNMT], [Bps[bk]], lambda e: e.matmul(ps[bk][:], Eall[:, kt // 2, :], NMT[:], start=False, stop=(not near)))
                        if near:
                            u0 = 512 * qc - 128 * kt + 384
                            OP("tensor", [Bc, BW], [Bps[bk]], lambda e: e.matmul(ps[bk][:], identb[:], Wn[:, u0:u0 + 512], start=False, stop=True))

                    emit_S(0)
                    for kt in range(nkt):
                        if kt + 1 < nkt:
                            emit_S(kt + 1)
                        bk = 2 + (kt % 2)
                        pslot = pscnt % 3; pscnt += 1
                        OP("scalar", [Bps[bk]], [BPT[pslot]], lambda e, bk=bk, pslot=pslot: e.activation(PT[pslot][:], ps[bk][:], AF.Exp, scale=SC))
                        OP("tensor", [BKV, BPT[pslot]], [Bps[4]], lambda e, kt=kt, pslot=pslot, nkt=nkt: e.matmul(ps[4][:], Vs[:, kt, :], PT[pslot][:], start=(kt == 0), stop=(kt == nkt - 1)))
                        if kt == 0:
                            OP("gpsimd", [BPT[pslot]], [Bacc], lambda e, pslot=pslot: e.tensor_copy(acc[:], PT[pslot][:]))
                        else:
                            OP("gpsimd", [BPT[pslot], Bacc], [Bacc], lambda e, pslot=pslot: e.tensor_tensor(acc[:], acc[:], PT[pslot][:], ALU.add))
                    OP("tensor", [Bc, Bacc], [Bps[5]], lambda e: e.matmul(ps[5][:], ones_f[:], acc[:], start=True, stop=True))
                    OP("vector", [Bps[5]], [Brinv], lambda e: e.reciprocal(rinv[:], ps[5][:]))
                    OP("vector", [Bps[4], Brinv], [Bob[s]], lambda e, s=s: e.tensor_tensor(ob[s][:], ps[4][:], rinv[:], ALU.mult))
                    P.dma("sync", lambda e, hh=hh, qc=qc, s=s: e.dma_start(out=AGINv[hh * 128:(hh + 1) * 128, qc // 2, (qc % 2) * 512:(qc % 2) * 512 + 512], in_=ob[s][:]),
                          reads=[Bob[s]], writes=[dB["AGIN"]], semc=sob[s])
            P.replay()

        with ExitStack() as ph:
            def sb(name, shape, dt):
                return ph.enter_context(nc.sbuf_tensor("a3_" + name, list(shape), dt))
            Bc = Buf("c3")
            DT = sb("DT", [128, 4, 512], F32); QD = sb("QD", [128, 512], F32); CD = sb("CD", [128, 1], F32)
            ones_f = sb("ones", [128, 128], F32)
            for (dst, src) in ((DT, DTd), (QD, QDd), (CD, CDd)):
                P.dma("sync", lambda e, dst=dst, src=src: e.dma_start(out=dst[:], in_=src), writes=[Bc], semc=None)
            OP("vector", [], [Bc], lambda e: e.memset(ones_f[:], 1.0))
            stf = sb("stf", [128, 2, 256], F32); stb = sb("stb", [128, 2, 256], BF16); Bst = Buf("st"); Bstb = Buf("stb")
            OP("vector", [], [Bst], lambda e: e.memset(stf[:], 0.0))
            OP("vector", [], [Bstb], lambda e: e.memset(stb[:], 0.0))
            Qs = [sb("Q%d" % i, [128, 2, 512], BF16) for i in range(2)]; Ks = [sb("K%d" % i, [128, 2, 512], BF16) for i in range(2)]
            Kds = [sb("Kd%d" % i, [128, 4, 256], BF16) for i in range(2)]; Vs = [sb("V%d" % i, [128, 4, 256], BF16) for i in range(2)]
            Gs = [sb("G%d" % i, [128, 2, 512], F32) for i in range(2)]
            Bin = [Buf("in0"), Buf("in1")]; sin_ = [P.new_sem("in0"), P.new_sem("in1")]
            Qd = sb("Qd", [128, 2, 512], BF16); BQd = Buf("Qd")
            inT = sb("inT", [128, 4, 512], BF16); BinT = Buf("inT")
            osb = sb("osb", [128, 2, 512], F32); osq = sb("osq", [128, 2, 512], F32); Bos = Buf("os")
            mean = sb("mean", [128, 512], F32); var = sb("var", [128, 512], F32); t1 = sb("t1", [128, 512], F32); Bmv = Buf("mv")
            sg = sb("sg", [128, 2, 512], F32); Bsg = Buf("sg")
            yb = [sb("yb%d" % i, [128, 2, 512], BF16) for i in range(2)]; Byb = [Buf("y0"), Buf("y1")]; syb = [P.new_sem("y0"), P.new_sem("y1")]
            RQTv = RQT.rearrange("h p t -> p h t"); RKTv = RKT.rearrange("h p t -> p h t"); RGTv = RGT.rearrange("h p t -> p h t")
            for n in range(16):
                s = n % 2
                t0 = n * 512
                P.dma("sync", lambda e, s=s, t0=t0: e.dma_start(out=Qs[s][:], in_=RQTv[:, :, t0:t0 + 512]), reads=[dB["RQT"]], writes=[Bin[s]], semc=sin_[s])
                P.dma("sync", lambda e, s=s, t0=t0: e.dma_start(out=Ks[s][:], in_=RKTv[:, :, t0:t0 + 512]), reads=[dB["RKT"]], writes=[Bin[s]], semc=sin_[s])
                P.dma("sync", lambda e, s=s, t0=t0: e.dma_start(out=Gs[s][:], in_=RGTv[:, :, t0:t0 + 512]), reads=[dB["RGT"]], writes=[Bin[s]], semc=sin_[s])
                P.dma("sync", lambda e, s=s, t0=t0: e.dma_start(out=Kds[s][:], in_=RKd[t0:t0 + 512, :].rearrange("(a p) d -> p a d", p=128)), reads=[dB["RKd"]], writes=[Bin[s]], semc=sin_[s])
                P.dma("sync", lambda e, s=s, t0=t0: e.dma_start(out=Vs[s][:], in_=RV[t0:t0 + 512, :].rearrange("(a p) d -> p a d", p=128)), reads=[dB["RV"]], writes=[Bin[s]], semc=sin_[s])
                OP("vector", [Bin[s], Bc], [BQd], lambda e, s=s: e.tensor_tensor(Qd[:], Qs[s][:], QD[:].unsqueeze(1).broadcast_to([128, 2, 512]), ALU.mult))
                for jt in range(4):
                    bk = jt % 2
                    for dt in range(2):
                        OP("tensor", [Bin[s]], [Bps[bk]], lambda e, jt=jt, dt=dt, bk=bk, s=s: e.matmul(ps[bk][:], Ks[s][:, dt, jt * 128:(jt + 1) * 128], Qs[s][:, dt, :], start=(dt == 0), stop=(dt == 1)))
                    OP("vector", [Bps[bk], Bc], [BinT], lambda e, jt=jt, bk=bk: e.tensor_tensor(inT[:, jt, :], ps[bk][:], DT[:, jt, :], ALU.mult))
                for et in range(2):
                    bk = 2 + et
                    for jt in range(4):
                        OP("tensor", [Bin[s], BinT], [Bps[bk]], lambda e, jt=jt, et=et, bk=bk, s=s: e.matmul(ps[bk][:], Vs[s][:, jt, et * 128:(et + 1) * 128], inT[:, jt, :], start=(jt == 0), stop=False))
                    for dt in range(2):
                        OP("tensor", [Bstb, BQd], [Bps[bk]], lambda e, dt=dt, et=et, bk=bk: e.matmul(ps[bk][:], stb[:, dt, et * 128:(et + 1) * 128], Qd[:, dt, :], start=False, stop=(dt == 1)))
                for dt in range(2):
                    bk = 4 + dt
                    for jt in range(4):
                        OP("tensor", [Bin[s]], [Bps[bk]], lambda e, jt=jt, dt=dt, bk=bk, s=s: e.matmul(ps[bk][:, 0:256], Kds[s][:, jt, dt * 128:(dt + 1) * 128], Vs[s][:, jt, :], start=(jt == 0), stop=(jt == 3)))
                    OP("vector", [Bps[bk], Bst, Bc], [Bst], lambda e, dt=dt, bk=bk: e.scalar_tensor_tensor(stf[:, dt, :], stf[:, dt, :], CD[:, 0:1], ps[bk][:, 0:256], ALU.mult, ALU.add))
                OP("vector", [Bst], [Bstb], lambda e: e.tensor_copy(stb[:], stf[:]))
                for et in range(2):
                    OP("scalar", [Bps[2 + et]], [Bos], lambda e, et=et: e.activation(osb[:, et, :], ps[2 + et][:], AF.Copy))
                    OP("scalar", [Bps[2 + et]], [Bos], lambda e, et=et: e.activation(osq[:, et, :], ps[2 + et][:], AF.Square))
                for et in range(2):
                    OP("tensor", [Bos, Bc], [Bps[6]], lambda e, et=et: e.matmul(ps[6][:], ones_f[:], osb[:, et, :], start=(et == 0), stop=(et == 1)))
                for et in range(2):
                    OP("tensor", [Bos, Bc], [Bps[7]], lambda e, et=et: e.matmul(ps[7][:], ones_f[:], osq[:, et, :], start=(et == 0), stop=(et == 1)))
                OP("vector", [Bps[6]], [Bmv], lambda e: e.tensor_scalar(mean[:], ps[6][:], 1.0 / 256, None, ALU.mult))
                OP("vector", [Bmv], [Bmv], lambda e: e.tensor_tensor(t1[:], mean[:], mean[:], ALU.mult))
                OP("vector", [Bps[7], Bmv], [Bmv], lambda e: e.scalar_tensor_tensor(var[:], ps[7][:], 1.0 / 256, t1[:], ALU.mult, ALU.subtract))
                OP("vector", [Bmv], [Bmv], lambda e: e.tensor_scalar(var[:], var[:], 1e-6, None, ALU.add))
                OP("scalar", [Bmv], [Bmv], lambda e: e.activation(t1[:], var[:], AF.Sqrt))
                OP("vector", [Bmv], [Bmv], lambda e: e.reciprocal(var[:], t1[:]))
                OP("scalar", [Bin[s]], [Bsg], lambda e, s=s: e.activation(sg[:], Gs[s][:], AF.Silu))
                for et in range(2):
                    OP("vector", [Bos, Bmv], [Bos], lambda e, et=et: e.tensor_tensor(osb[:, et, :], osb[:, et, :], mean[:], ALU.subtract))
                    OP("vector", [Bos, Bmv], [Bos], lambda e, et=et: e.tensor_tensor(osb[:, et, :], osb[:, et, :], var[:], ALU.mult))
                    OP("vector", [Bos, Bsg], [Byb[s]], lambda e, et=et, s=s: e.tensor_tensor(yb[s][:, et, :], osb[:, et, :], sg[:, et, :], ALU.mult))
                    P.dma("sync", lambda e, et=et, n=n, s=s: e.dma_start(out=AGINv[256 + et * 128:256 + (et + 1) * 128, n // 2, (n % 2) * 512:(n % 2) * 512 + 512], in_=yb[s][:, et, :]),
                          reads=[Byb[s]], writes=[dB["AGIN"]], semc=syb[s])
            P.replay()

        ccs = P.new_sem("cc")

        def cc_fn(e):
            return e.collective_compute("AllGather", ALU.bypass, replica_groups=[list(range(NCORE))],
                                        ins=[AGIN_t.ap().opt()], outs=[AGOUT_t.ap().opt()])
        P.dma("gpsimd", cc_fn, reads=[dB["AGIN"]], writes=[dB["AGOUT"]], semc=ccs, inc=1)
        P.replay()

        with ExitStack() as ph:
            def sb(name, shape, dt):
                return ph.enter_context(nc.sbuf_tensor("b1_" + name, list(shape), dt))
            mixT = sb("mixT", [128, 32, TC], BF16); Bmix = Buf("mix")
            gi = sb("gi", [128, 32], I32); Bgi = Buf("gi")
            g2_sb = sb("g2", [128, 32], F32)
            ones_b = sb("ones", [128, 128], BF16)
            P.dma("sync", lambda e: e.dma_start(out=gi[:], in_=gidx[:, :]), writes=[Bgi], semc=None)
            P.dma("sync", lambda e: e.dma_start(out=g2_sb[:], in_=g2[:, :]), writes=[Bgi], semc=None)
            OP("vector", [], [Bgi], lambda e: e.memset(ones_b[:], 1.0))
            sg_ = P.new_sem("gath")
            for ft in range(32):
                P.dma("gpsimd", lambda e, ft=ft: e.indirect_dma_start(out=mixT[:, ft, :], out_offset=None, in_=AGOUT[:, :],
                                                                      in_offset=bass.IndirectOffsetOnAxis(ap=gi[:, ft:ft + 1], axis=0)),
                      reads=[dB["AGOUT"], Bgi], writes=[Bmix], semc=sg_)
            if DEBUG:
                P.dma("sync", lambda e: e.dma_start(out=DBGM[:, :, :], in_=mixT[:]), reads=[Bmix], semc=None)
            wo = [sb("wo%d" % i, [128, 32 * 128], BF16) for i in range(2)]; Bwo = [Buf("wo0"), Buf("wo1")]; swo = [P.new_sem("wo0"), P.new_sem("wo1")]
            xr = [sb("xr%d" % i, [128, TC], F32) for i in range(2)]; Bxr = [Buf("xr0"), Buf("xr1")]; sxr = [P.new_sem("xr0"), P.new_sem("xr1")]
            h2 = [sb("h2%d" % i, [128, TC], F32) for i in range(2)]; Bh2 = [Buf("h20"), Buf("h21")]; sh2 = [P.new_sem("h20"), P.new_sem("h21")]
            h2b = sb("h2b", [128, 32, TC], BF16); Bh2b = Buf("h2b")
            sq = [sb("sq%d" % i, [128, TC], BF16) for i in range(2)]; Bsq = [Buf("sq0"), Buf("sq1")]
            for dmt in range(32):
                s = dmt % 2
                P.dma("gpsimd", lambda e, dmt=dmt, s=s: e.dma_start(out=wo[s][:].rearrange("p (a b) -> p a b", b=2048), in_=wod[dmt].rearrange("p f j -> p (f j)").rearrange("p (a b) -> p a b", b=2048)),
                      writes=[Bwo[s]], semc=swo[s])
                P.dma("sync", lambda e, dmt=dmt, s=s: e.dma_start(out=xr[s][:], in_=xTc[dmt * 128:(dmt + 1) * 128, :]), writes=[Bxr[s]], semc=sxr[s])
                for tch in range(2):
                    bk = tch
                    for ft in range(32):
                        OP("tensor", [Bwo[s], Bmix], [Bps[bk]], lambda e, ft=ft, tch=tch, bk=bk, s=s: e.matmul(ps[bk][:], wo[s][:, ft * 128:(ft + 1) * 128], mixT[:, ft, tch * 512:(tch + 1) * 512], start=(ft == 0), stop=(ft == 31)))
                    OP("vector", [Bps[bk], Bxr[s]], [Bh2[s]], lambda e, tch=tch, bk=bk, s=s: e.tensor_tensor(h2[s][:, tch * 512:(tch + 1) * 512], ps[bk][:], xr[s][:, tch * 512:(tch + 1) * 512], ALU.add))
                OP("scalar", [Bh2[s]], [Bsq[s]], lambda e, s=s: e.activation(sq[s][:], h2[s][:], AF.Square))
                OP("gpsimd", [Bh2[s]], [Bh2b], lambda e, s=s, dmt=dmt: e.tensor_copy(h2b[:, dmt, :], h2[s][:]))
                for tch in range(2):
                    OP("tensor", [Bsq[s], Bgi], [Bps[2 + tch]], lambda e, tch=tch, s=s, dmt=dmt: e.matmul(ps[2 + tch][:], ones_b[:], sq[s][:, tch * 512:(tch + 1) * 512], start=(dmt == 0), stop=(dmt == 31)))
                P.dma("sync", lambda e, dmt=dmt, s=s: e.dma_start(out=H2T[dmt * 128:(dmt + 1) * 128, :], in_=h2[s][:]), reads=[Bh2[s]], writes=[dB["H2T"]], semc=sh2[s])
            rs2 = sb("rs2", [128, TC], F32); rs2b = sb("rs2b", [128, TC], F32); Brs2 = Buf("rs2")
            for tch in range(2):
                OP("vector", [Bps[2 + tch]], [Brs2], lambda e, tch=tch: e.tensor_scalar(rs2[:, tch * 512:(tch + 1) * 512], ps[2 + tch][:], 1.0 / D, 1e-6, ALU.mult, ALU.add))
            OP("scalar", [Brs2], [Brs2], lambda e: e.activation(rs2b[:], rs2[:], AF.Sqrt))
            OP("vector", [Brs2], [Brs2], lambda e: e.reciprocal(rs2[:], rs2b[:]))
            xn = [sb("xn%d" % i, [128, TC], BF16) for i in range(2)]; Bxn = [Buf("xn0"), Buf("xn1")]; sxn = [P.new_sem("xn0"), P.new_sem("xn1")]
            for dmt in range(32):
                s = dmt % 2
                OP("vector", [Bh2b, Brs2, Bgi], [Bxn[s]], lambda e, dmt=dmt, s=s: e.scalar_tensor_tensor(xn[s][:], h2b[:, dmt, :], g2_sb[:, dmt:dmt + 1], rs2[:], ALU.mult, ALU.mult))
                P.dma("sync", lambda e, dmt=dmt, s=s: e.dma_start(out=XN2T[dmt * 128:(dmt + 1) * 128, :], in_=xn[s][:]), reads=[Bxn[s]], writes=[dB["XN2T"]], semc=sxn[s])
            P.replay()

        XN2Tv = XN2T.rearrange("(k p) t -> p k t", p=128)

        with ExitStack() as keep:
            def sbk(name, shape, dt):
                return keep.enter_context(nc.sbuf_tensor("pk_" + name, list(shape), dt))
            IDX1T = sbk("IDX1T", [128, TC], F32); IDX2T = sbk("IDX2T", [128, TC], F32); GATET = sbk("GATET", [128, TC], F32)
            BIG = Buf("IG")
            with ExitStack() as ph:
                def sb(name, shape, dt):
                    return ph.enter_context(nc.sbuf_tensor("b2_" + name, list(shape), dt))
                Bc = Buf("c5")
                ident = sb("ident", [128, 128], F32); iota = sb("iota", [128, 128], F32); keyT = sb("keyT", [128, 16, 128], BF16)
                P.dma("sync", lambda e: e.dma_start(out=ident[:], in_=identd[:, :]), writes=[Bc], semc=None)
                P.dma("sync", lambda e: e.dma_start(out=iota[:], in_=iotad[:, :]), writes=[Bc], semc=None)
                P.dma("gpsimd", lambda e: e.dma_start(out=keyT[:], in_=keyTd[:, :, :]), writes=[Bc], semc=None)
                xn2 = sb("xn2", [128, 32, TC], BF16); Bxn2 = Buf("xn2")
                for q4 in range(4):
                    P.dma("sync", lambda e, q4=q4: e.dma_start(out=xn2[:, q4 * 8:(q4 + 1) * 8, :], in_=XN2Tv[:, q4 * 8:(q4 + 1) * 8, :]), reads=[dB["XN2T"]], writes=[Bxn2], semc=None)
                wq = [sb("wq%d" % i, [128, 32 * 128], BF16) for i in range(2)]; Bwq = [Buf("wq0"), Buf("wq1")]; swq = [P.new_sem("wq0"), P.new_sem("wq1")]
                qT = sb("qT", [128, 16, TC], BF16); BqT = Buf("qT")
                for hc in range(16):
                    s = hc % 2
                    P.dma("gpsimd", lambda e, hc=hc, s=s: e.dma_start(out=wq[s][:].rearrange("p (a b) -> p a b", b=2048), in_=wqd[hc].rearrange("p f j -> p (f j)").rearrange("p (a b) -> p a b", b=2048)),
                          writes=[Bwq[s]], semc=swq[s])
                    for tch in range(2):
                        bk = tch
                        for kt in range(32):
                            OP("tensor", [Bwq[s], Bxn2], [Bps[bk]], lambda e, kt=kt, tch=tch, bk=bk, s=s: e.matmul(ps[bk][:], wq[s][:, kt * 128:(kt + 1) * 128], xn2[:, kt, tch * 512:(tch + 1) * 512], start=(kt == 0), stop=(kt == 31)))
                        OP("scalar", [Bps[bk]], [BqT], lambda e, hc=hc, tch=tch, bk=bk: e.activation(qT[:, hc, tch * 512:(tch + 1) * 512], ps[bk][:], AF.Copy))
                ssb = sb("ssb", [128, 16, 128], F32); Bss = Buf("ss")
                Bssl = [Buf("ss%d" % i) for i in range(16)]; Bsvl = [Buf("sv%d" % i) for i in range(16)]; Bsil = [Buf("si%d" % i) for i in range(16)]
                Bcl = [Buf("c%d" % i) for i in range(8)]; Btl = [Buf("t%d" % i) for i in range(8)]; Bpl = [Buf("p%d" % i) for i in range(8)]
                sv = sb("sv", [128, 16, 16], F32); siu = sb("siu", [128, 16, 16], U32); sif = sb("sif", [128, 16, 16], F32); Bsv = Buf("sv")
                cand = sb("cand", [128, 8, 256], F32); Bcand = Buf("cand")
                tops = sb("tops", [128, 8, 16], F32); posu = sb("posu", [128, 8, 16], U32); posf = sb("posf", [128, 8, 16], F32); Btop = Buf("top")
                af = sb("af", [128, 8, 16], F32); bf = sb("bf", [128, 8, 16], F32); au = sb("au", [128, 8, 16], U32); bu = sb("bu", [128, 8, 16], U32)
                oh = sb("oh", [128, 8, 16, 16], F32); Boh = Buf("oh")
                idx1 = sb("idx1", [128, 128], F32); idx2 = sb("idx2", [128, 128], F32); gate = sb("gate", [128, 128], F32); Big = Buf("ig")
                zz = sb("zz", [128, 8], F32)
                for tt in range(8):
                    for hc in range(16):
                        bk = 2 + hc // 4
                        OP("tensor", [BqT, Bc], [Bps[bk]], lambda e, hc=hc, tt=tt, bk=bk: e.matmul(ps[bk][:, (hc % 4) * 128:(hc % 4 + 1) * 128], qT[:, hc, tt * 128:(tt + 1) * 128], keyT[:, hc, :], start=True, stop=True))
                    for g in range(4):
                        OP("scalar", [Bps[2 + g]], Bssl[g * 4:(g + 1) * 4], lambda e, g=g: e.activation(ssb[:, g * 4:(g + 1) * 4, :], ps[2 + g][:].rearrange("p (a b) -> p a b", b=128), AF.Copy))
                    for hc in range(16):
                        OP("vector", [Bssl[hc]], [Bsvl[hc]], lambda e, hc=hc: e.max(sv[:, hc, 0:8], ssb[:, hc, :]))
                    for hc in range(16):
                        OP("vector", [Bssl[hc], Bsvl[hc]], [Bsil[hc]], lambda e, hc=hc: e.max_index(siu[:, hc, 0:8], sv[:, hc, 0:8], ssb[:, hc, :]))
                    for hc in range(16):
                        OP("vector", [Bssl[hc], Bsvl[hc]], [Bssl[hc]], lambda e, hc=hc: e.match_replace(ssb[:, hc, :], sv[:, hc, 0:8], ssb[:, hc, :], -1e30))
                    for hc in range(16):
                        OP("vector", [Bssl[hc]], [Bsvl[hc]], lambda e, hc=hc: e.max(sv[:, hc, 8:16], ssb[:, hc, :]))
                    for hc in range(16):
                        OP("vector", [Bssl[hc], Bsvl[hc]], [Bsil[hc]], lambda e, hc=hc: e.max_index(siu[:, hc, 8:16], sv[:, hc, 8:16], ssb[:, hc, :]))
                    OP("vector", Bsil, [Bsv], lambda e: e.tensor_copy(sif[:], siu[:]))
                    svv = sv[:].rearrange("p (h c) k -> p h c k", c=2)
                    sifv = sif[:].rearrange("p (h c) k -> p h c k", c=2)
                    for h in range(8):
                        OP("vector", [Bsvl[2 * h], Bsvl[2 * h + 1]], [Bcl[h]], lambda e, h=h, svv=svv: e.tensor_tensor(cand[:, h, :].rearrange("p (a b) -> p a b", b=16),
                                                                                 svv[:, h, 0, :].unsqueeze(2).broadcast_to([128, 16, 16]),
                                                                                 svv[:, h, 1, :].unsqueeze(1).broadcast_to([128, 16, 16]), ALU.add))
                    for h in range(8):
                        OP("vector", [Bcl[h]], [Btl[h]], lambda e, h=h: e.max(tops[:, h, 0:8], cand[:, h, :]))
                    for h in range(8):
                        OP("vector", [Bcl[h], Btl[h]], [Bpl[h]], lambda e, h=h: e.max_index(posu[:, h, 0:8], tops[:, h, 0:8], cand[:, h, :]))
                    for h in range(8):
                        OP("vector", [Bcl[h], Btl[h]], [Bcl[h]], lambda e, h=h: e.match_replace(cand[:, h, :], tops[:, h, 0:8], cand[:, h, :], -1e30))
                    for h in range(8):
                        OP("vector", [Bcl[h]], [Btl[h]], lambda e, h=h: e.max(tops[:, h, 8:16], cand[:, h, :]))
                    for h in range(8):
                        OP("vector", [Bcl[h], Btl[h]], [Bpl[h]], lambda e, h=h: e.max_index(posu[:, h, 8:16], tops[:, h, 8:16], cand[:, h, :]))
                    OP("vector", Bpl + Btl, [Btop], lambda e: e.tensor_scalar(au[:], posu[:], 4, None, ALU.logical_shift_right))
                    OP("vector", [Btop], [Btop], lambda e: e.tensor_scalar(bu[:], posu[:], 15, None, ALU.bitwise_and))
                    OP("vector", [Btop], [Btop], lambda e: e.tensor_copy(af[:], au[:]))
                    OP("vector", [Btop], [Btop], lambda e: e.tensor_copy(bf[:], bu[:]))
                    for (sel_, c, dst) in ((af, 0, idx1), (bf, 1, idx2)):
                        for h in range(8):
                            OP("vector", [Btop, Bc], [Boh], lambda e, h=h, sel_=sel_: e.tensor_tensor(oh[:, h, :, :], iota[:, 0:16].unsqueeze(1).broadcast_to([128, 16, 16]),
                                                                                         sel_[:, h, :].unsqueeze(2).broadcast_to([128, 16, 16]), ALU.is_equal))
                            OP("vector", [Boh, Bsv], [Boh], lambda e, h=h, c=c, sifv=sifv: e.tensor_tensor(oh[:, h, :, :], oh[:, h, :, :], sifv[:, h, c, :].unsqueeze(1).broadcast_to([128, 16, 16]), ALU.mult))
                        OP("vector", [Boh], [Big], lambda e, dst=dst: e.tensor_reduce(dst[:], oh[:].rearrange("p h k a -> p (h k) a"), AX.X, ALU.add))
                    OP("vector", [Btop] + Btl, [Btop], lambda e: e.tensor_tensor(posf[:], tops[:], tops[:, :, 0:1].broadcast_to([128, 8, 16]), ALU.subtract))
                    OP("scalar", [Btop], [Btop], lambda e: e.activation(posf[:], posf[:], AF.Exp))
                    OP("vector", [Btop], [Btop], lambda e: e.tensor_reduce(zz[:], posf[:], AX.X, ALU.add))
                    OP("vector", [Btop], [Btop], lambda e: e.reciprocal(zz[:], zz[:]))
                    OP("vector", [Btop], [Big], lambda e: e.tensor_tensor(gate[:].rearrange("p (h k) -> p h k", k=16), posf[:], zz[:].unsqueeze(2).broadcast_to([128, 8, 16]), ALU.mult))
                    for j, (src, dstT) in enumerate(((idx1, IDX1T), (idx2, IDX2T), (gate, GATET))):
                        bk = 6 + (j % 2)
                        OP("tensor", [Big, Bc], [Bps[bk]], lambda e, src=src, bk=bk: e.transpose(ps[bk][:, 0:128], src[:], ident[:]))
                        OP("vector", [Bps[bk]], [BIG], lambda e, dstT=dstT, tt=tt, bk=bk: e.tensor_copy(dstT[:, tt * 128:(tt + 1) * 128], ps[bk][:, 0:128]))
                P.replay()

            Gd = dscr("Gd", [128, 128, TC], BF16); Wd = dscr("Wd", [128, 128, TC], BF16)
            BGd = Buf("Gd"); BWd = Buf("Wd")
            Gdv = Gd.rearrange("i j t -> j i t")
            with ExitStack() as ph:
                def sb(name, shape, dt):
                    return ph.enter_context(nc.sbuf_tensor("b3a_" + name, list(shape), dt))
                Bc = Buf("c6")
                iotaf = sb("iotaf", [128, 128], F32); iotab = sb("iotab", [128, 128], BF16)
                P.dma("sync", lambda e: e.dma_start(out=iotaf[:], in_=iotad[:, :]), writes=[Bc], semc=None)
                OP("vector", [Bc], [Bc], lambda e: e.tensor_copy(iotab[:], iotaf[:]))
                TT = 256
                NSB = 32
                Gst = [sb("Gst%d" % i, [128, 128, TT], BF16) for i in range(2)]; BGst = [Buf("g0"), Buf("g1")]; sGst = [P.new_sem("g0"), P.new_sem("g1")]
                Lr = [sb("L%d" % i, [128, NSB, 128], BF16) for i in range(2)]; Rr = [sb("R%d" % i, [128, NSB, 128], BF16) for i in range(2)]
                BL = [Buf("l0"), Buf("l1")]; BR = [Buf("r0"), Buf("r1")]
                gcnt = 0
                for T in range(TC // TT):
                    tb = T * TT
                    gs = T % 2
                    for sbk_ in range(TT // NSB):
                        s = sbk_ % 2
                        c0 = tb + sbk_ * NSB
                        for tl in range(NSB):
                            OP("vector", [Bc, BIG], [BL[s]], lambda e, s=s, tl=tl, c0=c0: e.tensor_scalar(Lr[s][:, tl, :], iotab[:], IDX1T[:, c0 + tl:c0 + tl + 1], GATET[:, c0 + tl:c0 + tl + 1], ALU.is_equal, ALU.mult))
                            OP("vector", [Bc, BIG], [BR[s]], lambda e, s=s, tl=tl, c0=c0: e.tensor_scalar(Rr[s][:, tl, :], iotab[:], IDX2T[:, c0 + tl:c0 + tl + 1], None, ALU.is_equal))
                        for g16 in range(NSB // 16):
                            half = gcnt % 2; gcnt += 1
                            pst = psA if half == 0 else psB
                            for k in range(16):
                                tl = g16 * 16 + k
                                OP("tensor", [BL[s], BR[s]], [Bps[half * 4 + k // 4]], lambda e, s=s, tl=tl, k=k, pst=pst: e.matmul(pst[:, k * 128:(k + 1) * 128], Rr[s][:, tl, :], Lr[s][:, tl, :], start=True, stop=True))
                            tloc = sbk_ * NSB + g16 * 16
                            OP("scalar", [Bps[half * 4 + q] for q in range(4)], [BGst[gs]], lambda e, pst=pst, tloc=tloc, gs=gs: e.activation(Gst[gs][:, :, tloc:tloc + 16], pst[:, :].rearrange("p (t i) -> p i t", i=128), AF.Copy))
                    for i8 in range(8):
                        P.dma("sync", lambda e, i8=i8, gs=gs, tb=tb: e.dma_start(out=Gdv[:, i8 * 16:(i8 + 1) * 16, tb:tb + TT], in_=Gst[gs][:, i8 * 16:(i8 + 1) * 16, :]),
                              reads=[BGst[gs]], writes=[BGd], semc=sGst[gs])
                P.replay()
            with ExitStack() as ph:
                def sb(name, shape, dt):
                    return ph.enter_context(nc.sbuf_tensor("b3b_" + name, list(shape), dt))
                xn2 = sb("xn2", [128, 32, TC], BF16); Bxn2 = Buf("xn2")
                for q4 in range(4):
                    P.dma("sync", lambda e, q4=q4: e.dma_start(out=xn2[:, q4 * 8:(q4 + 1) * 8, :], in_=XN2Tv[:, q4 * 8:(q4 + 1) * 8, :]), reads=[dB["XN2T"]], writes=[Bxn2], semc=None)
                UTs = [sb("UT%d" % i, [128, 32 * 128], BF16) for i in range(3)]; BUT = [Buf("u%d" % i) for i in range(3)]; sUT = [P.new_sem("u%d" % i) for i in range(3)]
                Gi = [sb("Gi%d" % i, [128, TC], BF16) for i in range(3)]; BGi = [Buf("gi%d" % i) for i in range(3)]; sGi = [P.new_sem("gi%d" % i) for i in range(3)]
                ag = [sb("ag%d" % i, [128, TC], F32) for i in range(2)]; Bag = [Buf("ag0"), Buf("ag1")]
                Wst = [sb("Wst%d" % i, [128, TC], BF16) for i in range(2)]; BWst = [Buf("w0"), Buf("w1")]; sWst = [P.new_sem("w0"), P.new_sem("w1")]
                for i in range(128):
                    s3 = i % 3; s = i % 2
                    P.dma("gpsimd", lambda e, i=i, s3=s3: e.dma_start(out=UTs[s3][:].rearrange("p (a b) -> p a b", b=2048), in_=UTd[i].rearrange("p (a b) -> p a b", b=2048)), writes=[BUT[s3]], semc=sUT[s3])
                    P.dma("sync", lambda e, i=i, s3=s3: e.dma_start(out=Gi[s3][:], in_=Gd[i, :, :]), reads=[BGd], writes=[BGi[s3]], semc=sGi[s3])
                    for tch in range(2):
                        bk = (i % 2) * 2 + tch
                        for kt in range(32):
                            OP("tensor", [BUT[s3], Bxn2], [Bps[bk]], lambda e, kt=kt, bk=bk, s3=s3, tch=tch: e.matmul(ps[bk][:], UTs[s3][:, kt * 128:(kt + 1) * 128], xn2[:, kt, tch * 512:(tch + 1) * 512], start=(kt == 0), stop=(kt == 31)))
                        OP("scalar", [Bps[bk]], [Bag[s]], lambda e, bk=bk, s=s, tch=tch: e.activation(ag[s][:, tch * 512:(tch + 1) * 512], ps[bk][:], AF.Gelu))
                    OP("vector", [Bag[s], BGi[s3]], [BWst[s]], lambda e, s=s, s3=s3: e.tensor_tensor(Wst[s][:], ag[s][:], Gi[s3][:], ALU.mult))
                    P.dma("sync", lambda e, i=i, s=s: e.dma_start(out=Wd[i, :, :], in_=Wst[s][:]), reads=[BWst[s]], writes=[BWd], semc=sWst[s])
                P.replay()
            with ExitStack() as ph:
                def sb(name, shape, dt):
                    return ph.enter_context(nc.sbuf_tensor("b3c_" + name, list(shape), dt))
                Wi = [sb("Wi%d" % i, [128, TC], BF16) for i in range(4)]; BWi = [Buf("wi%d" % i) for i in range(4)]; sWi = [P.new_sem("wi%d" % i) for i in range(4)]
                Vh = [sb("Vh%d" % i, [128, 512], BF16) for i in range(4)]; BVh = [Buf("v%d" % i) for i in range(4)]; sVh = [P.new_sem("v%d" % i) for i in range(4)]
                h2r = sb("h2r", [128, 4, TC], F32); Bh2r = Buf("h2r"); sh2r = P.new_sem("h2r")
                h3 = sb("h3", [128, 4, TC], F32); Bh3 = Buf("h3"); sh3 = P.new_sem("h3")
                H2Tv = H2T.rearrange("(k p) t -> p k t", p=128); H3Tv = H3T.rearrange("(k p) t -> p k t", p=128)
                cnt = 0
                NRES = 64
                Wres = sb("Wres", [128, NRES, TC], BF16); BWres = Buf("Wres"); sWres = P.new_sem("wres")
                Wdv = Wd.rearrange("i j t -> j i t")
                for r8 in range(NRES // 8):
                    P.dma("sync", lambda e, r8=r8: e.dma_start(out=Wres[:, r8 * 8:(r8 + 1) * 8, :], in_=Wdv[:, r8 * 8:(r8 + 1) * 8, :]), reads=[BWd], writes=[BWres], semc=sWres)
                for sw in range(8):
                    P.dma("sync", lambda e, sw=sw: e.dma_start(out=h2r[:], in_=H2Tv[:, sw * 4:(sw + 1) * 4, :]), reads=[dB["H2T"]], writes=[Bh2r], semc=sh2r)
                    for i in range(128):
                        s = cnt % 4; cnt += 1
                        if i >= NRES:
                            P.dma("sync", lambda e, i=i, s=s: e.dma_start(out=Wi[s][:], in_=Wd[i, :, :]), reads=[BWd], writes=[BWi[s]], semc=sWi[s])
                        P.dma("gpsimd", lambda e, i=i, s=s, sw=sw: e.dma_start(out=Vh[s][:], in_=Vd[i * 128:(i + 1) * 128, sw * 512:(sw + 1) * 512]), writes=[BVh[s]], semc=sVh[s])
                        for dmt in range(4):
                            for tch in range(2):
                                bk = dmt * 2 + tch
                                if i < NRES:
                                    OP("tensor", [BVh[s], BWres], [Bps[bk]], lambda e, dmt=dmt, tch=tch, s=s, bk=bk, i=i: e.matmul(ps[bk][:], Vh[s][:, dmt * 128:(dmt + 1) * 128], Wres[:, i, tch * 512:(tch + 1) * 512], start=(i == 0), stop=(i == 127)))
                                else:
                                    OP("tensor", [BVh[s], BWi[s]], [Bps[bk]], lambda e, dmt=dmt, tch=tch, s=s, bk=bk, i=i: e.matmul(ps[bk][:], Vh[s][:, dmt * 128:(dmt + 1) * 128], Wi[s][:, tch * 512:(tch + 1) * 512], start=(i == 0), stop=(i == 127)))
                    for dmt in range(4):
                        for tch in range(2):
                            bk = dmt * 2 + tch
                            OP("vector", [Bps[bk], Bh2r], [Bh3], lambda e, bk=bk, dmt=dmt, tch=tch: e.tensor_tensor(h3[:, dmt, tch * 512:(tch + 1) * 512], ps[bk][:], h2r[:, dmt, tch * 512:(tch + 1) * 512], ALU.add))
                    P.dma("sync", lambda e, sw=sw: e.dma_start(out=H3Tv[:, sw * 4:(sw + 1) * 4, :], in_=h3[:]), reads=[Bh3], writes=[dB["H3T"]], semc=sh3)
                P.replay()

        with ExitStack() as ph:
            def sb(name, shape, dt):
                return ph.enter_context(nc.sbuf_tensor("b4_" + name, list(shape), dt))
            Bc = Buf("c7")
            gf_sb = sb("gf", [128, 32], F32); ones_b = sb("ones", [128, 128], BF16)
            P.dma("sync", lambda e: e.dma_start(out=gf_sb[:], in_=gf[:, :]), writes=[Bc], semc=None)
            OP("vector", [], [Bc], lambda e: e.memset(ones_b[:], 1.0))
            hh3 = [sb("h%d" % i, [128, 32, 256], F32) for i in range(2)]; Bhh = [Buf("h0"), Buf("h1")]; shh = [P.new_sem("h0"), P.new_sem("h1")]
            sqq = sb("sqq", [128, 32, 256], BF16); Bsqq = Buf("sqq")
            r1 = sb("r1", [128, 256], F32); r2 = sb("r2", [128, 256], F32); Br = Buf("r")
            yo = [sb("yo%d" % i, [128, 8, 256], F32) for i in range(2)]; Byo = [Buf("yo0"), Buf("yo1")]; syo = [P.new_sem("yo0"), P.new_sem("yo1")]
            H3Tv = H3T.rearrange("(k p) t -> p k t", p=128); YTv = YT.rearrange("(k p) t -> p k t", p=128)
            yc = 0
            for T in range(4):
                s = T % 2
                tb = T * 256
                P.dma("sync", lambda e, s=s, tb=tb: e.dma_start(out=hh3[s][:], in_=H3Tv[:, :, tb:tb + 256]), reads=[dB["H3T"]], writes=[Bhh[s]], semc=shh[s])
                OP("scalar", [Bhh[s]], [Bsqq], lambda e, s=s: e.activation(sqq[:], hh3[s][:], AF.Square))
                for kt in range(32):
                    OP("tensor", [Bsqq, Bc], [Bps[0]], lambda e, kt=kt: e.matmul(ps[0][:, 0:256], ones_b[:], sqq[:, kt, :], start=(kt == 0), stop=(kt == 31)))
                OP("vector", [Bps[0]], [Br], lambda e: e.tensor_scalar(r1[:], ps[0][:, 0:256], 1.0 / D, 1e-6, ALU.mult, ALU.add))
                OP("scalar", [Br], [Br], lambda e: e.activation(r2[:], r1[:], AF.Sqrt))
                OP("vector", [Br], [Br], lambda e: e.reciprocal(r1[:], r2[:]))
                for q in range(4):
                    ys = yc % 2; yc += 1
                    for k in range(8):
                        kt = q * 8 + k
                        OP("vector", [Bhh[s], Br, Bc], [Byo[ys]], lambda e, kt=kt, k=k, s=s, ys=ys: e.scalar_tensor_tensor(yo[ys][:, k, :], hh3[s][:, kt, :], gf_sb[:, kt:kt + 1], r1[:], ALU.mult, ALU.mult))
                    P.dma("sync", lambda e, q=q, ys=ys, tb=tb: e.dma_start(out=YTv[:, q * 8:(q + 1) * 8, tb:tb + 256], in_=yo[ys][:]), reads=[Byo[ys]], semc=syo[ys])
            P.replay()
    return nc


def _t5_bucket(n):
    n = np.maximum(n, 0)
    nf = np.maximum(n, 1).astype(np.float32)
    large = 16 + (np.log(nf / np.float32(16)) / np.float32(math.log(128 / 16)) * np.float32(16)).astype(np.int32)
    large = np.minimum(large, 31)
    return np.where(n < 16, n, large)


def _host_consts():
    c = {}
    half = 128
    inv = (np.float32(10000.0) ** (-np.arange(half, dtype=np.float32) / np.float32(half))).astype(np.float32)
    ang = np.arange(S, dtype=np.float32)[:, None] * inv[None, :]
    cos = np.cos(ang).astype(np.float32); sin = np.sin(ang).astype(np.float32)
    c["cosT"] = np.ascontiguousarray(cos.T); c["sinT"] = np.ascontiguousarray(sin.T)
    qt = np.arange(64)[:, None]; blk = np.arange(32)[None, :]
    cur = qt // 2
    CB = np.where(blk < cur, 0.0, -1e30).astype(np.float32)
    PAST = (blk < cur).astype(np.float32)
    OWN = (blk == cur).astype(np.float32)
    for nm, a in (("CB", CB), ("PAST", PAST), ("OWN", OWN)):
        c[nm] = np.ascontiguousarray(np.broadcast_to(a[None], (128, 64, 32))).astype(np.float32)
    E = np.zeros((32, 32, 128), np.float32)
    for b in range(32):
        E[b, b, :] = 1.0
    c["Eall"] = E.astype(ml_dtypes.bfloat16)
    k = np.arange(128)[:, None]; u = np.arange(1024)[None, :]
    dist = u - 384 - k
    bucket = _t5_bucket(dist)
    OH = np.zeros((128, 31, 1024), np.float32)
    for b in range(31):
        OH[:, b, :] = ((bucket == b) & (dist >= 0))
    c["OH"] = OH.astype(ml_dtypes.bfloat16)
    c["CAUSW"] = np.where(dist >= 0, 0.0, NEGM).astype(np.float32)
    c["identf"] = np.eye(128, dtype=np.float32)
    c["iota128"] = np.ascontiguousarray(np.broadcast_to(np.arange(128, dtype=np.float32)[None], (128, 128)))
    return c


def _ret_consts(h):
    g = np.float64(1.0) - np.float64(2.0) ** (-5.0 - h)
    lg = np.log(g)
    j = np.arange(512)
    jj = (np.arange(4)[None, :, None] * 128 + np.arange(128)[:, None, None])
    ii = np.arange(512)[None, None, :]
    diff = ii - jj
    DT = np.where(diff >= 0, np.exp(lg * np.maximum(diff, 0)), 0.0).astype(np.float32)
    QD = np.broadcast_to(np.exp(lg * (j + 1.0))[None, :], (128, 512)).astype(np.float32)
    CD = np.full((128, 1), np.exp(lg * 512.0), np.float32)
    t = np.arange(64)[None, :] * 128 + np.arange(128)[:, None]
    kd = np.exp(lg * (511.0 - (t % 512))).astype(np.float32)
    return np.ascontiguousarray(DT), np.ascontiguousarray(QD), CD, np.ascontiguousarray(kd)


def _tile_w(w):
    C = w.shape[1]
    return np.ascontiguousarray(w.reshape(32, 128, C).transpose(1, 0, 2))


def _vecpk(v):
    return np.ascontiguousarray(v.reshape(32, 128).T.astype(np.float32))


_CACHE = {}


def kernel(x, norm_mix_g, w_in, w_out, rel_bias, norm_ffn_g, peer_w_q, peer_sub_keys, peer_u, peer_v, norm_final_g):
    x = np.asarray(x, np.float32)[0]
    w_in = np.asarray(w_in, np.float32)[0]; w_out = np.asarray(w_out, np.float32)[0]
    rel_bias = np.asarray(rel_bias, np.float32)
    wq = np.asarray(peer_w_q, np.float32)[0]; sk = np.asarray(peer_sub_keys, np.float32)[0]
    u = np.asarray(peer_u, np.float32)[0]; v = np.asarray(peer_v, np.float32)[0]
    if "nc" not in _CACHE:
        _CACHE["nc"] = build_nc()
        _CACHE["c"] = _host_consts()
    nc = _CACHE["nc"]; C = _CACHE["c"]
    xT = np.ascontiguousarray(x.T)
    shared = dict(C)
    shared["xT"] = xT
    shared["g1"] = _vecpk(np.asarray(norm_mix_g)[0]); shared["g2"] = _vecpk(np.asarray(norm_ffn_g)[0]); shared["gf"] = _vecpk(np.asarray(norm_final_g))
    perm = np.concatenate([np.concatenate([256 * r + np.arange(256), 2048 + 256 * r + np.arange(256)]) for r in range(8)])
    wo_p = w_out[perm]
    shared["wo"] = np.ascontiguousarray(wo_p.reshape(32, 128, 32, 128).transpose(2, 1, 0, 3))
    shared["wq"] = np.ascontiguousarray(wq.reshape(32, 128, 16, 128).transpose(2, 1, 0, 3))
    shared["keyT"] = np.ascontiguousarray(sk.reshape(16, 128, 128).transpose(2, 0, 1))
    shared["UT"] = np.ascontiguousarray(u.reshape(128, 128, 32, 128).transpose(0, 3, 2, 1)).reshape(128, 128, 4096)
    shared["V"] = np.ascontiguousarray(v)
    in_maps = []
    for c in range(NCORE):
        m = dict(shared)
        m["xTc"] = np.ascontiguousarray(xT[:, c * TC:(c + 1) * TC])
        h0, h1 = 2 * c, 2 * c + 1
        mq = lambda h: np.arange(h * 128, (h + 1) * 128)
        fm_cols = np.concatenate([mq(h0), mq(h1), 2048 + mq(h0), 2048 + mq(h1),
                                  6144 + c * 256 + np.arange(256), 8192 + c * 256 + np.arange(256), 12288 + c * 256 + np.arange(256)])
        tm_cols = np.concatenate([4096 + mq(h0), 4096 + mq(h1), 10240 + c * 256 + np.arange(256)])
        m["wfm"] = _tile_w(w_in[:, fm_cols]); m["wtm"] = _tile_w(w_in[:, tm_cols])
        DT, QD, CD, kd = _ret_consts(c)
        m["DT"] = DT; m["QD"] = QD; m["CD"] = CD; m["kdec"] = kd
        m["rbc"] = np.ascontiguousarray(rel_bias[:, [h0, h1]].T)
        fg = np.arange(32)[None, :] * 128 + np.arange(128)[:, None]
        m["gidx"] = ((fg // 512) * 4096 + (fg % 512) * 8 + c).astype(np.int32)
        in_maps.append(m)
    res = run_bass_kernel_spmd(nc, in_maps, core_ids=list(range(NCORE)))
    _CACHE["res"] = res
    y = np.concatenate([np.asarray(r["YT"]) for r in res.results], axis=1)
    return np.ascontiguousarray(y.T).reshape(1, S, D).astype(np.float32)
```

```python
import math
from contextlib import ExitStack
import numpy as np
import ml_dtypes
import concourse.bass as bass
import concourse.mybir as mybir
from concourse.bass_utils import run_bass_kernel_spmd

F32 = mybir.dt.float32
BF16 = mybir.dt.bfloat16
U32 = mybir.dt.uint32
I32 = mybir.dt.int32
AF = mybir.ActivationFunctionType
ALU = mybir.AluOpType
AX = mybir.AxisListType

ENGINES = ("tensor", "vector", "scalar", "gpsimd", "sync")
COMPUTE = ("tensor", "vector", "scalar", "gpsimd")
S = 8192
D = 4096
NCORE = 8
TC = 1024
NEGM = -30000.0
DEBUG = False


class Buf:
    __slots__ = ("name", "last_write", "readers")

    def __init__(self, name):
        self.name = name
        self.last_write = None
        self.readers = []


def _compact(readers):
    best = {}
    for sem, val, src in readers:
        k = id(sem)
        if k not in best or best[k][1] < val:
            best[k] = (sem, val, src)
    return list(best.values())


class Prog:
    def __init__(self, nc, stack):
        self.nc = nc
        self.q = {e: [] for e in ENGINES}
        self.cnt = {e: 0 for e in COMPUTE}
        self.known = {e: {} for e in ENGINES}
        self.fin = {}
        self._stack = stack
        self.esem = {e: stack.enter_context(nc.semaphore("cnt_" + e)) for e in COMPUTE}
        self.nsem = 0
        self.free1 = []
        self.used1 = []

    def new_sem(self, name=None):
        self.nsem += 1
        s = self._stack.enter_context(self.nc.semaphore("s%d_%s" % (self.nsem, name or "")))
        return [s, 0]

    def _need(self, eng, deps):
        out = {}
        kn = self.known[eng]
        for d in deps:
            if d is None:
                continue
            sem, val, src = d
            if src == "tensor" and eng == "tensor":
                continue
            k = id(sem)
            if kn.get(k, 0) >= val:
                continue
            if k not in out or out[k][1] < val:
                out[k] = (sem, val)
        for k, (sem, val) in out.items():
            kn[k] = val
        return list(out.values())

    def _deps(self, reads, writes):
        deps = []
        for b in reads:
            deps.append(b.last_write)
        for b in writes:
            deps.append(b.last_write)
            deps.extend(b.readers)
        return deps

    def _mark(self, tok, reads, writes):
        for b in reads:
            b.readers.append(tok)
            if len(b.readers) > 6:
                b.readers = _compact(b.readers)
        for b in writes:
            b.last_write = tok
            b.readers = []

    def op(self, eng, fn, reads=(), writes=()):
        waits = self._need(eng, self._deps(reads, writes))
        self.cnt[eng] += 1
        sem = self.esem[eng]
        self.q[eng].append((fn, waits, (sem, 1)))
        tok = (sem, self.cnt[eng], eng)
        self._mark(tok, reads, writes)
        return tok

    def dma(self, eng, fn, reads=(), writes=(), semc=None, inc=16):
        if semc is None:
            if self.free1:
                semc = self.free1.pop()
            else:
                semc = self.new_sem("d")
            self.used1.append(semc)
        waits = self._need(eng, self._deps(reads, writes))
        semc[1] += inc
        sem, val = semc[0], semc[1]
        self.q[eng].append((fn, waits, (sem, inc)))
        tok = (sem, val, None)
        self._mark(tok, reads, writes)
        k = id(sem)
        if k not in self.fin or self.fin[k][1] < val:
            self.fin[k] = (sem, val)
        return tok

    def barrier(self):
        allw = [(s, v, None) for (s, v) in self.fin.values()]
        for e in COMPUTE:
            if self.cnt[e] > 0:
                allw.append((self.esem[e], self.cnt[e], None))
        for e in ENGINES:
            w = self._need(e, allw)
            if w:
                self.q[e].append((None, w, None))

    def replay(self):
        self.barrier()
        with self.nc.Block() as block:
            for e in ENGINES:
                items = self.q[e]

                def body(engine, items=items):
                    for fn, waits, inc in items:
                        for sem, val in waits:
                            engine.wait_ge(sem, val)
                        if fn is not None:
                            ins = fn(engine)
                            if inc is not None:
                                ins.then_inc(inc[0], inc[1])

                getattr(block, e)(body)
        self.q = {e: [] for e in ENGINES}
        self.free1.extend(self.used1)
        self.used1 = []


def build_nc():
    nc = bass.Bass("TRN2", target_bir_lowering=False)

    def din(name, shape, dt=F32):
        return nc.dram_tensor(name, list(shape), dt, kind="ExternalInput").ap()

    def dscr(name, shape, dt):
        return nc.dram_tensor(name, list(shape), dt).ap()

    xT = din("xT", [D, S]); xTc = din("xTc", [D, TC])
    g1 = din("g1", [128, 32]); g2 = din("g2", [128, 32]); gf = din("gf", [128, 32])
    wfm = din("wfm", [128, 32, 1280]); wtm = din("wtm", [128, 32, 512])
    cosT = din("cosT", [128, S]); sinT = din("sinT", [128, S])
    kdec = din("kdec", [128, 64]); DTd = din("DT", [128, 4, 512]); QDd = din("QD", [128, 512]); CDd = din("CD", [128, 1])
    rbc = din("rbc", [2, 32])
    CBd = din("CB", [128, 64, 32]); PASTd = din("PAST", [128, 64, 32]); OWNd = din("OWN", [128, 64, 32])
    Ealld = din("Eall", [32, 32, 128], BF16); OHd = din("OH", [128, 31, 1024], BF16); CAUSd = din("CAUSW", [128, 1024])
    identd = din("identf", [128, 128])
    wod = din("wo", [32, 128, 32, 128]); gidx = din("gidx", [128, 32], I32)
    wqd = din("wq", [16, 128, 32, 128]); keyTd = din("keyT", [128, 16, 128])
    UTd = din("UT", [128, 128, 32 * 128]); Vd = din("V", [16384, D])
    iotad = din("iota128", [128, 128])
    YT = nc.dram_tensor("YT", [D, TC], F32, kind="ExternalOutput").ap()

    MQT = dscr("MQT", [2, 128, S], BF16); MKT = dscr("MKT", [2, 128, S], BF16); MV = dscr("MV", [S, 256], BF16)
    RQT = dscr("RQT", [2, 128, S], BF16); RKT = dscr("RKT", [2, 128, S], BF16); RGT = dscr("RGT", [2, 128, S], F32)
    RKd = dscr("RKd", [S, 256], BF16); RV = dscr("RV", [S, 256], BF16)
    AGI_t = [nc.dram_tensor("AGI0", [128 * 8, 1024], BF16), nc.dram_tensor("AGI1", [128 * 8, 1024], BF16), nc.dram_tensor("AGIr", [256 * 8, 1024], BF16)]
    AGO_t = [nc.dram_tensor("AGO0", [8 * 1024, 1024], BF16), nc.dram_tensor("AGO1", [8 * 1024, 1024], BF16), nc.dram_tensor("AGOr", [8 * 2048, 1024], BF16)]
    AGIv = [t.ap().rearrange("(f b) t -> f b t", b=8) for t in AGI_t]
    AGO = [t.ap() for t in AGO_t]
    if DEBUG:
        H2T = nc.dram_tensor("H2T", [D, TC], F32, kind="ExternalOutput").ap()
        DBGM = nc.dram_tensor("DBGM", [128, 32, TC], BF16, kind="ExternalOutput").ap()
    else:
        H2T = dscr("H2T", [D, TC], F32)
    XN2T = dscr("XN2T", [D, TC], BF16); H3T = dscr("H3T", [D, TC], F32)

    SC = 128 ** -0.5

    with ExitStack() as top:
        P = Prog(nc, top)
        psA = top.enter_context(nc.psum_tensor("psA", [128, 2048], F32))
        psB = top.enter_context(nc.psum_tensor("psB", [128, 2048], F32))
        ps = [psA[:, i * 512:(i + 1) * 512] for i in range(4)] + [psB[:, i * 512:(i + 1) * 512] for i in range(4)]
        Bps = [Buf("ps%d" % i) for i in range(8)]
        WnP = [top.enter_context(nc.sbuf_tensor("WnP%d" % h, [128, 1024], BF16)) for h in range(2)]; BWnP = [Buf("WnP0"), Buf("WnP1")]
        c1P = [top.enter_context(nc.sbuf_tensor("c1P%d" % h, [128, 1], F32)) for h in range(2)]; BWc1 = [Buf("c1P0"), Buf("c1P1")]
        dB = {n: Buf(n) for n in "MQT MKT MV RQT RKT RGT RKd RV AGI0 AGI1 AGIr AGO0 AGO1 AGOr H2T XN2T H3T".split()}
        ccsem = [P.new_sem("cc0"), P.new_sem("cc1"), P.new_sem("ccr")]

        def allgather(k):
            nm = ("0", "1", "r")[k]

            def cc_fn(e):
                return e.collective_compute("AllGather", ALU.bypass, replica_groups=[list(range(NCORE))],
                                            ins=[AGI_t[k].ap().opt()], outs=[AGO_t[k].ap().opt()])
            P.dma("gpsimd", cc_fn, reads=[dB["AGI" + nm]], writes=[dB["AGO" + nm]], semc=ccsem[k], inc=1)


        def OP(eng, reads, writes, f):
            return P.op(eng, f, reads=reads, writes=writes)

        with ExitStack() as ph:
            def sb(name, shape, dt):
                return ph.enter_context(nc.sbuf_tensor("a0_" + name, list(shape), dt))
            Bc0 = Buf("c0")
            OH = sb("OH", [128, 31, 1024], BF16); CAUS = sb("CAUS", [128, 1024], F32)
            P.dma("sync", lambda e: e.dma_start(out=CAUS[:], in_=CAUSd), writes=[Bc0], semc=None)
            for q4 in range(4):
                lo = q4 * 8; hi = min(31, lo + 8)
                P.dma("sync", lambda e, lo=lo, hi=hi: e.dma_start(out=OH[:, lo:hi, :], in_=OHd[:, lo:hi, :]), writes=[Bc0], semc=None)
            Wf = sb("Wf", [128, 1024], F32); BWf = Buf("Wf")
            for hh in range(2):
                rb = sb("rb%d" % hh, [128, 32], F32); rbrel = sb("rbrel%d" % hh, [128, 32], F32); Brb0 = Buf("rb")
                P.dma("sync", lambda e, hh=hh, rb=rb: e.dma_start(out=rb[:], in_=rbc[hh:hh + 1, :].partition_broadcast(128)), writes=[Brb0], semc=None)
                OP("vector", [Brb0], [Brb0], lambda e, rb=rb, rbrel=rbrel: e.tensor_scalar(rbrel[:], rb[:], rb[:, 31:32], 1.0 / SC, ALU.subtract, ALU.mult))
                OP("vector", [Brb0], [BWc1[hh]], lambda e, rb=rb, hh=hh: e.tensor_scalar(c1P[hh][:], rb[:, 31:32], 1.0 / SC, -NEGM, ALU.mult, ALU.add))
                OP("vector", [Bc0, BWnP[hh]], [BWf], lambda e: e.tensor_copy(Wf[:], CAUS[:]))
                for b in range(31):
                    OP("vector", [Bc0, Brb0, BWf], [BWf], lambda e, b=b, rbrel=rbrel: e.scalar_tensor_tensor(Wf[:], OH[:, b, :], rbrel[:, b:b + 1], Wf[:], ALU.mult, ALU.add))
                OP("vector", [BWf], [BWnP[hh]], lambda e, hh=hh: e.tensor_copy(WnP[hh][:], Wf[:]))
            P.replay()

        with ExitStack() as ph:
            def sb(name, shape, dt):
                return ph.enter_context(nc.sbuf_tensor("a1_" + name, list(shape), dt))
            wfm_sb = sb("wfm", [128, 32, 1280], BF16); wtm_sb = sb("wtm", [128, 32, 512], BF16)
            Bw = Buf("w")
            wst0 = sb("wst0", [128, 1280], F32); wst = [wst0, wst0]; Bwst0 = Buf("wst0"); Bwst = [Bwst0, Bwst0]
            swst0 = P.new_sem("wst0"); swst = [swst0, swst0]
            g1_sb = sb("g1", [128, 32], F32); Bg1 = Buf("g1")
            kdec_sb = sb("kdec", [128, 64], F32)
            ones_b = sb("ones", [128, 128], BF16); Bones = Buf("ones")
            ident = sb("ident", [128, 128], F32); Bid = Buf("ident")
            P.dma("sync", lambda e: e.dma_start(out=g1_sb[:], in_=g1[:, :]), writes=[Bg1], semc=None)
            P.dma("sync", lambda e: e.dma_start(out=kdec_sb[:], in_=kdec[:, :]), writes=[Bg1], semc=None)
            P.dma("sync", lambda e: e.dma_start(out=ident[:], in_=identd[:, :]), writes=[Bid], semc=None)
            OP("vector", [], [Bones], lambda e: e.memset(ones_b[:], 1.0))
            n = 0
            for (src, dst, width) in ((wfm, wfm_sb, 1280), (wtm, wtm_sb, 512)):
                for kt in range(32):
                    s = n % 2; n += 1
                    P.dma("sync", lambda e, s=s, kt=kt, src=src, width=width: e.dma_start(out=wst[s][:, 0:width], in_=src[:, kt, :]),
                          writes=[Bwst[s]], semc=swst[s])
                    OP("vector", [Bwst[s], Bg1], [Bw],
                       lambda e, s=s, kt=kt, dst=dst, width=width: e.tensor_scalar(dst[:, kt, :], wst[s][:, 0:width], g1_sb[:, kt:kt + 1], None, ALU.mult))
            xb = [sb("xb%d" % i, [128, 32, 256], BF16) for i in range(2)]; Bxb = [Buf("xb0"), Buf("xb1")]
            sxb = [P.new_sem("xb0"), P.new_sem("xb1")]
            xsq = sb("xsq", [128, 32, 256], BF16); Bxsq = Buf("xsq")
            cst = [sb("cst%d" % i, [128, 2, 256], F32) for i in range(2)]; Bcst = [Buf("c0"), Buf("c1")]
            sc_ = [P.new_sem("cs0"), P.new_sem("cs1")]
            rsA = sb("rsA", [128, 256], F32); rsB = sb("rsB", [128, 256], F32); rstd = sb("rstd", [128, 256], F32)
            Brs = Buf("rs"); Brstd = Buf("rstd")
            rstt = sb("rstt", [128, 2], F32); Brstt = Buf("rstt")
            qk_st = [sb("qk%d" % i, [128, 8, 256], BF16) for i in range(2)]; Bqk = [Buf("qk0"), Buf("qk1")]
            g_st = [sb("gs%d" % i, [128, 2, 256], F32) for i in range(2)]; Bgs = [Buf("gs0"), Buf("gs1")]
            rot = sb("rot", [128, 4, 256], F32); Brot = Buf("rot")
            tmp = sb("tmp", [128, 4, 256], F32); Btmp = Buf("tmp")
            tm_st = [[sb("tm%d_%d" % (i, j), [128, 3, 256], BF16) for j in range(2)] for i in range(2)]
            Btm = [[Buf("tm"), Buf("tm")], [Buf("tm"), Buf("tm")]]
            identb = sb("identb", [128, 128], BF16)
            OP("vector", [Bid], [Bid], lambda e: e.tensor_copy(identb[:], ident[:]))
            psb6 = ps[6][:].bitcast(BF16)
            stm_ = [[P.new_sem("tm"), P.new_sem("tm")], [P.new_sem("tm"), P.new_sem("tm")]]
            sqk_ = [P.new_sem("qk0"), P.new_sem("qk1")]; sgs_ = [P.new_sem("gs0"), P.new_sem("gs1")]
            xTv = xT.rearrange("(k p) t -> p k t", p=128)
            MQTv = MQT.rearrange("h p t -> p h t"); MKTv = MKT.rearrange("h p t -> p h t")
            RQTv = RQT.rearrange("h p t -> p h t"); RKTv = RKT.rearrange("h p t -> p h t"); RGTv = RGT.rearrange("h p t -> p h t")
            NCH = S // 256
            for tc in range(NCH):
                s = tc % 2
                t0 = tc * 256
                P.dma("gpsimd", lambda e, s=s, t0=t0: e.dma_start(out=xb[s][:], in_=xTv[:, :, t0:t0 + 256]), writes=[Bxb[s]], semc=sxb[s])
                P.dma("sync", lambda e, s=s, t0=t0: e.dma_start(out=cst[s][:, 0, :], in_=cosT[:, t0:t0 + 256]), writes=[Bcst[s]], semc=sc_[s])
                P.dma("sync", lambda e, s=s, t0=t0: e.dma_start(out=cst[s][:, 1, :], in_=sinT[:, t0:t0 + 256]), writes=[Bcst[s]], semc=sc_[s])
                OP("scalar", [Bxb[s]], [Bxsq], lambda e, s=s: e.activation(xsq[:], xb[s][:], AF.Square))
                for kt in range(32):
                    OP("tensor", [Bxsq, Bones], [Bps[0]], lambda e, kt=kt: e.matmul(ps[0][:, 0:256], ones_b[:], xsq[:, kt, :], start=(kt == 0), stop=(kt == 31)))
                OP("vector", [Bps[0]], [Brs], lambda e: e.tensor_scalar(rsA[:], ps[0][:, 0:256], 1.0 / D, 1e-6, ALU.mult, ALU.add))
                OP("scalar", [Brs], [Brs], lambda e: e.activation(rsB[:], rsA[:], AF.Sqrt))
                OP("vector", [Brs], [Brstd], lambda e: e.reciprocal(rstd[:], rsB[:]))
                for tt in range(2):
                    OP("tensor", [Brstd, Bid], [Bps[1]], lambda e, tt=tt: e.transpose(ps[1][:, tt * 128:(tt + 1) * 128], rstd[:, tt * 128:(tt + 1) * 128], ident[:]))
                OP("vector", [Bps[1]], [Brstt], lambda e: e.tensor_copy(rstt[:], ps[1][:, 0:256].rearrange("p (a b) -> p a b", b=128)[:, :, 0]))
                for ct in range(10):
                    bk = 2 + ct % 3
                    for kt in range(32):
                        OP("tensor", [Bxb[s], Bw], [Bps[bk]], lambda e, kt=kt, ct=ct, bk=bk, s=s: e.matmul(ps[bk][:, 0:256], wfm_sb[:, kt, ct * 128:(ct + 1) * 128], xb[s][:, kt, :], start=(kt == 0), stop=(kt == 31)))
                    if ct < 4:
                        OP("vector", [Bps[bk], Brstd], [Bqk[s]], lambda e, ct=ct, bk=bk, s=s: e.tensor_tensor(qk_st[s][:, ct, :], ps[bk][:, 0:256], rstd[:], ALU.mult))
                    elif ct < 6:
                        OP("vector", [Bps[bk], Brstd], [Brot], lambda e, ct=ct, bk=bk: e.tensor_tensor(rot[:, ct - 4, :], ps[bk][:, 0:256], rstd[:], ALU.mult))
                    elif ct < 8:
                        OP("vector", [Bps[bk], Brstd], [Brot], lambda e, ct=ct, bk=bk: e.scalar_tensor_tensor(rot[:, ct - 4, :], ps[bk][:, 0:256], 0.0625, rstd[:], ALU.mult, ALU.mult))
                    else:
                        OP("vector", [Bps[bk], Brstd], [Bgs[s]], lambda e, ct=ct, bk=bk, s=s: e.tensor_tensor(g_st[s][:, ct - 8, :], ps[bk][:, 0:256], rstd[:], ALU.mult))
                for a, dst in ((0, 4), (2, 6)):
                    OP("vector", [Brot, Bcst[s]], [Btmp], lambda e, a=a, s=s: e.tensor_tensor(tmp[:, 0, :], rot[:, a, :], cst[s][:, 0, :], ALU.mult))
                    OP("vector", [Brot, Bcst[s]], [Btmp], lambda e, a=a, s=s: e.tensor_tensor(tmp[:, 1, :], rot[:, a + 1, :], cst[s][:, 1, :], ALU.mult))
                    OP("vector", [Brot, Bcst[s]], [Btmp], lambda e, a=a, s=s: e.tensor_tensor(tmp[:, 2, :], rot[:, a, :], cst[s][:, 1, :], ALU.mult))
                    OP("vector", [Brot, Bcst[s]], [Btmp], lambda e, a=a, s=s: e.tensor_tensor(tmp[:, 3, :], rot[:, a + 1, :], cst[s][:, 0, :], ALU.mult))
                    OP("vector", [Btmp], [Bqk[s]], lambda e, dst=dst, s=s: e.tensor_tensor(qk_st[s][:, dst, :], tmp[:, 0, :], tmp[:, 1, :], ALU.subtract))
                    OP("vector", [Btmp], [Bqk[s]], lambda e, dst=dst, s=s: e.tensor_tensor(qk_st[s][:, dst + 1, :], tmp[:, 2, :], tmp[:, 3, :], ALU.add))
                for tt in range(2):
                    gt = tc * 2 + tt
                    for kt in range(32):
                        OP("tensor", [Bxb[s], Bw], [Bps[5]], lambda e, kt=kt, tt=tt, s=s: e.matmul(ps[5][:, 0:512], xb[s][:, kt, tt * 128:(tt + 1) * 128], wtm_sb[:, kt, 0:512], start=(kt == 0), stop=(kt == 31)))
                    OP("scalar", [Bps[5], Brstt], [Btm[s][tt]], lambda e, tt=tt, s=s: e.activation(tm_st[s][tt][:, 0, :], ps[5][:, 0:256], AF.Copy, scale=rstt[:, tt:tt + 1]))
                    OP("scalar", [Bps[5], Brstt], [Btm[s][tt]], lambda e, tt=tt, s=s: e.activation(tm_st[s][tt][:, 2, :], ps[5][:, 256:512], AF.Copy, scale=rstt[:, tt:tt + 1]))
                    for hf in range(2):
                        OP("tensor", [Bqk[s], Bid], [Bps[6]], lambda e, tt=tt, s=s, hf=hf: e.transpose(psb6[:, hf * 128:(hf + 1) * 128], qk_st[s][:, 6 + hf, tt * 128:(tt + 1) * 128], identb[:]))
                    OP("vector", [Bps[6], Bg1], [Btm[s][tt]], lambda e, tt=tt, s=s, gt=gt: e.tensor_scalar(tm_st[s][tt][:, 1, :], psb6[:, 0:256], kdec_sb[:, gt:gt + 1], None, ALU.mult))
                    r0 = t0 + tt * 128
                    P.dma("sync", lambda e, tt=tt, s=s, r0=r0: e.dma_start(out=MV[r0:r0 + 128, :], in_=tm_st[s][tt][:, 0, :]), reads=[Btm[s][tt]], writes=[dB["MV"]], semc=stm_[s][tt])
                    P.dma("sync", lambda e, tt=tt, s=s, r0=r0: e.dma_start(out=RKd[r0:r0 + 128, :], in_=tm_st[s][tt][:, 1, :]), reads=[Btm[s][tt]], writes=[dB["RKd"]], semc=stm_[s][tt])
                    P.dma("sync", lambda e, tt=tt, s=s, r0=r0: e.dma_start(out=RV[r0:r0 + 128, :], in_=tm_st[s][tt][:, 2, :]), reads=[Btm[s][tt]], writes=[dB["RV"]], semc=stm_[s][tt])
                for (dv, lo, nm) in ((MQTv, 0, "MQT"), (MKTv, 2, "MKT"), (RQTv, 4, "RQT"), (RKTv, 6, "RKT")):
                    P.dma("sync", lambda e, dv=dv, lo=lo, s=s, t0=t0: e.dma_start(out=dv[:, :, t0:t0 + 256], in_=qk_st[s][:, lo:lo + 2, :]), reads=[Bqk[s]], writes=[dB[nm]], semc=sqk_[s])
                P.dma("sync", lambda e, s=s, t0=t0: e.dma_start(out=RGTv[:, :, t0:t0 + 256], in_=g_st[s][:]), reads=[Bgs[s]], writes=[dB["RGT"]], semc=sgs_[s])
            P.replay()


        with ExitStack() as ph:
            def sb(name, shape, dt):
                return ph.enter_context(nc.sbuf_tensor("a2_" + name, list(shape), dt))
            ones_b = sb("ones", [128, 128], BF16); identb = sb("identb", [128, 128], BF16); ident = sb("ident", [128, 128], F32)
            Bc = Buf("consts")
            CB = sb("CB", [128, 64, 32], F32); PAST = sb("PAST", [128, 64, 32], F32); OWN = sb("OWN", [128, 64, 32], F32)
            Eall = sb("Eall", [32, 32, 128], BF16)
            for (dst, src) in ((CB, CBd), (PAST, PASTd), (OWN, OWNd), (Eall, Ealld), (ident, identd)):
                P.dma("sync", lambda e, dst=dst, src=src: e.dma_start(out=dst[:], in_=src), writes=[Bc], semc=None)
            OP("vector", [], [Bc], lambda e: e.memset(ones_b[:], 1.0))
            OP("vector", [Bc], [Bc], lambda e: e.tensor_copy(identb[:], ident[:]))
            KTs = [sb("KT%d" % h, [128, S], BF16) for h in range(2)]; Vss = [sb("V%d" % h, [128, 64, 128], BF16) for h in range(2)]
            BKVs = [Buf("KV0"), Buf("KV1")]
            c1s = c1P; Brbs = BWc1; Wns = WnP; BWs = BWnP
            kmf = sb("kmf", [128, 32], F32); kmTs = [sb("kmT%d" % h, [128, 32], BF16) for h in range(2)]; Bkms = [Buf("km0"), Buf("km1")]
            QT = [sb("QT%d" % i, [128, 512], BF16) for i in range(2)]; BQ = [Buf("q0"), Buf("q1")]; sQ = [P.new_sem("q0"), P.new_sem("q1")]
            gm = sb("gm", [128, 4, 32], F32); m8 = sb("m8", [128, 4, 8], F32); sel = sb("sel", [128, 4, 32], F32); NM = sb("NM", [128, 4, 32], F32)
            Bgm = Buf("gm"); Bm8 = Buf("m8"); Bsel = Buf("sel"); BNM = Buf("NM")
            NMT = sb("NMT", [32, 512], BF16); BNMT = Buf("NMT")
            PT = [sb("PT%d" % i, [128, 512], BF16) for i in range(3)]; BPT = [Buf("pt0"), Buf("pt1"), Buf("pt2")]
            rinv = sb("rinv", [128, 512], F32); Brinv = Buf("rinv")
            ob = [sb("ob%d" % i, [128, 512], BF16) for i in range(2)]; Bob = [Buf("ob0"), Buf("ob1")]; sob = [P.new_sem("ob0"), P.new_sem("ob1")]
            for hh in range(2):
                P.dma("sync", lambda e, hh=hh: e.dma_start(out=KTs[hh][:], in_=MKT[hh, :, :]), reads=[dB["MKT"]], writes=[BKVs[hh]], semc=None)
                P.dma("sync", lambda e, hh=hh: e.dma_start(out=Vss[hh][:], in_=MV[:, hh * 128:(hh + 1) * 128].rearrange("(k p) d -> p k d", p=128)), reads=[dB["MV"]], writes=[BKVs[hh]], semc=None)
            for hh in range(2):
                OP("vector", [BKVs[hh]], [Bkms[hh]], lambda e, hh=hh: e.tensor_reduce(kmf[:], KTs[hh][:].rearrange("p (b k) -> p b k", k=256), AX.X, ALU.add))
                OP("vector", [Bkms[hh]], [Bkms[hh]], lambda e, hh=hh: e.tensor_scalar(kmTs[hh][:], kmf[:], 1.0 / 256, None, ALU.mult))
            def ret_phase():
                with ExitStack() as ph:
                    def sb(name, shape, dt):
                        return ph.enter_context(nc.sbuf_tensor("a3_" + name, list(shape), dt))
                    Bc = Buf("c3")
                    DT = sb("DT", [128, 4, 512], F32); QD = sb("QD", [128, 512], F32); CD = sb("CD", [128, 1], F32)
                    ones_f = sb("ones", [128, 128], F32)
                    for (dst, src) in ((DT, DTd), (QD, QDd), (CD, CDd)):
                        P.dma("sync", lambda e, dst=dst, src=src: e.dma_start(out=dst[:], in_=src), writes=[Bc], semc=None)
                    OP("vector", [], [Bc], lambda e: e.memset(ones_f[:], 1.0))
                    stf = sb("stf", [128, 2, 256], F32); stb = sb("stb", [128, 2, 256], BF16); Bst = Buf("st"); Bstb = Buf("stb")
                    OP("vector", [], [Bst], lambda e: e.memset(stf[:], 0.0))
                    OP("vector", [], [Bstb], lambda e: e.memset(stb[:], 0.0))
                    NB = 2
                    Qs = [sb("Q%d" % i, [128, 2, 512], BF16) for i in range(NB)]; Ks = [sb("K%d" % i, [128, 2, 512], BF16) for i in range(NB)]
                    Kds = [sb("Kd%d" % i, [128, 4, 256], BF16) for i in range(NB)]; Vs = [sb("V%d" % i, [128, 4, 256], BF16) for i in range(NB)]
                    Gs = [sb("G%d" % i, [128, 2, 512], F32) for i in range(NB)]
                    Bin = [Buf("in%d" % i) for i in range(NB)]; sin_ = [P.new_sem("in%d" % i) for i in range(NB)]
                    Qd = [sb("Qd%d" % i, [128, 2, 512], BF16) for i in range(2)]; BQd = [Buf("Qd0"), Buf("Qd1")]
                    inT = [sb("inT%d" % i, [128, 4, 512], BF16) for i in range(2)]; BinT = [Buf("inT0"), Buf("inT1")]
                    osb = [sb("osb%d" % i, [128, 2, 512], F32) for i in range(2)]; osq = [sb("osq%d" % i, [128, 2, 512], F32) for i in range(2)]; Bos = [Buf("os0"), Buf("os1")]
                    mean = sb("mean", [128, 512], F32); var = sb("var", [128, 512], F32); t1 = sb("t1", [128, 512], F32); Bmv = Buf("mv")
                    yb = [sb("yb%d" % i, [128, 2, 512], BF16) for i in range(2)]; Byb = [Buf("y0"), Buf("y1")]; syb = [P.new_sem("y0"), P.new_sem("y1")]
                    RQTv = RQT.rearrange("h p t -> p h t"); RKTv = RKT.rearrange("h p t -> p h t"); RGTv = RGT.rearrange("h p t -> p h t")

                    def stageA(n):
                        s = n % NB; d = n % 2
                        t0 = n * 512
                        P.dma("sync", lambda e: e.dma_start(out=Qs[s][:], in_=RQTv[:, :, t0:t0 + 512]), reads=[dB["RQT"]], writes=[Bin[s]], semc=sin_[s])
                        P.dma("sync", lambda e: e.dma_start(out=Ks[s][:], in_=RKTv[:, :, t0:t0 + 512]), reads=[dB["RKT"]], writes=[Bin[s]], semc=sin_[s])
                        P.dma("sync", lambda e: e.dma_start(out=Gs[s][:], in_=RGTv[:, :, t0:t0 + 512]), reads=[dB["RGT"]], writes=[Bin[s]], semc=sin_[s])
                        P.dma("sync", lambda e: e.dma_start(out=Kds[s][:], in_=RKd[t0:t0 + 512, :].rearrange("(a p) d -> p a d", p=128)), reads=[dB["RKd"]], writes=[Bin[s]], semc=sin_[s])
                        P.dma("sync", lambda e: e.dma_start(out=Vs[s][:], in_=RV[t0:t0 + 512, :].rearrange("(a p) d -> p a d", p=128)), reads=[dB["RV"]], writes=[Bin[s]], semc=sin_[s])
                        OP("vector", [Bin[s], Bc], [BQd[d]], lambda e: e.tensor_tensor(Qd[d][:], Qs[s][:], QD[:].unsqueeze(1).broadcast_to([128, 2, 512]), ALU.mult))
                        for jt in range(4):
                            bk = jt % 2
                            for dt in range(2):
                                OP("tensor", [Bin[s]], [Bps[bk]], lambda e, jt=jt, dt=dt, bk=bk: e.matmul(ps[bk][:], Ks[s][:, dt, jt * 128:(jt + 1) * 128], Qs[s][:, dt, :], start=(dt == 0), stop=(dt == 1)))
                            OP("vector", [Bps[bk], Bc], [BinT[d]], lambda e, jt=jt, bk=bk: e.tensor_tensor(inT[d][:, jt, :], ps[bk][:], DT[:, jt, :], ALU.mult))
                        for et in range(2):
                            bk = 2 + et
                            for jt in range(4):
                                OP("tensor", [Bin[s], BinT[d]], [Bps[bk]], lambda e, jt=jt, et=et, bk=bk: e.matmul(ps[bk][:], Vs[s][:, jt, et * 128:(et + 1) * 128], inT[d][:, jt, :], start=(jt == 0), stop=False))
                            for dt in range(2):
                                OP("tensor", [Bstb, BQd[d]], [Bps[bk]], lambda e, dt=dt, et=et, bk=bk: e.matmul(ps[bk][:], stb[:, dt, et * 128:(et + 1) * 128], Qd[d][:, dt, :], start=False, stop=(dt == 1)))
                        for dt in range(2):
                            bk = 4 + dt
                            for jt in range(4):
                                OP("tensor", [Bin[s]], [Bps[bk]], lambda e, jt=jt, dt=dt, bk=bk: e.matmul(ps[bk][:, 0:256], Kds[s][:, jt, dt * 128:(dt + 1) * 128], Vs[s][:, jt, :], start=(jt == 0), stop=(jt == 3)))
                            OP("vector", [Bps[bk], Bst, Bc], [Bst], lambda e, dt=dt, bk=bk: e.scalar_tensor_tensor(stf[:, dt, :], stf[:, dt, :], CD[:, 0:1], ps[bk][:, 0:256], ALU.mult, ALU.add))
                        OP("vector", [Bst], [Bstb], lambda e: e.tensor_copy(stb[:], stf[:]))
                        for et in range(2):
                            OP("scalar", [Bps[2 + et]], [Bos[d]], lambda e, et=et: e.activation(osb[d][:, et, :], ps[2 + et][:], AF.Copy))
                            OP("scalar", [Bps[2 + et]], [Bos[d]], lambda e, et=et: e.activation(osq[d][:, et, :], ps[2 + et][:], AF.Square))

                    def stageB(n):
                        s = n % NB; d = n % 2
                        for et in range(2):
                            OP("tensor", [Bos[d], Bc], [Bps[6]], lambda e, et=et: e.matmul(ps[6][:], ones_f[:], osb[d][:, et, :], start=(et == 0), stop=(et == 1)))
                        for et in range(2):
                            OP("tensor", [Bos[d], Bc], [Bps[7]], lambda e, et=et: e.matmul(ps[7][:], ones_f[:], osq[d][:, et, :], start=(et == 0), stop=(et == 1)))
                        OP("vector", [Bps[6]], [Bmv], lambda e: e.tensor_scalar(mean[:], ps[6][:], 1.0 / 256, None, ALU.mult))
                        OP("vector", [Bmv], [Bmv], lambda e: e.tensor_tensor(t1[:], mean[:], mean[:], ALU.mult))
                        OP("vector", [Bps[7], Bmv], [Bmv], lambda e: e.scalar_tensor_tensor(var[:], ps[7][:], 1.0 / 256, t1[:], ALU.mult, ALU.subtract))
                        OP("vector", [Bmv], [Bmv], lambda e: e.tensor_scalar(var[:], var[:], 1e-6, None, ALU.add))
                        OP("scalar", [Bmv], [Bmv], lambda e: e.activation(t1[:], var[:], AF.Sqrt))
                        OP("vector", [Bmv], [Bmv], lambda e: e.reciprocal(var[:], t1[:]))
                        OP("scalar", [Bin[s]], [Bin[s]], lambda e: e.activation(Gs[s][:], Gs[s][:], AF.Silu))
                        for et in range(2):
                            OP("vector", [Bos[d], Bmv], [Bos[d]], lambda e, et=et: e.tensor_tensor(osb[d][:, et, :], osb[d][:, et, :], mean[:], ALU.subtract))
                            OP("vector", [Bos[d], Bmv], [Bos[d]], lambda e, et=et: e.tensor_tensor(osb[d][:, et, :], osb[d][:, et, :], var[:], ALU.mult))
                            OP("vector", [Bos[d], Bin[s]], [Byb[d]], lambda e, et=et: e.tensor_tensor(yb[d][:, et, :], osb[d][:, et, :], Gs[s][:, et, :], ALU.mult))
                            P.dma("sync", lambda e, et=et: e.dma_start(out=AGIv[2][et * 128:(et + 1) * 128, n // 2, (n % 2) * 512:(n % 2) * 512 + 512], in_=yb[d][:, et, :]),
                                  reads=[Byb[d]], writes=[dB["AGIr"]], semc=syb[d])

                    stageA(0)
                    for n in range(16):
                        if n + 1 < 16:
                            stageA(n + 1)
                        stageB(n)
                    P.replay()


            ret_phase()
            pscnt = 0
            allgather(2)
            for hh in range(2):
                if hh == 1:
                    allgather(0)
                KT = KTs[hh]; Vs = Vss[hh]; BKV = BKVs[hh]; Wn = Wns[hh]; BW = BWs[hh]; kmT = kmTs[hh]; Bkm = Bkms[hh]; c1 = c1s[hh]; Brb = Brbs[hh]
                for qc in range(16):
                    s = qc % 2
                    P.dma("sync", lambda e, hh=hh, qc=qc, s=s: e.dma_start(out=QT[s][:], in_=MQT[hh, :, qc * 512:(qc + 1) * 512]), reads=[dB["MQT"]], writes=[BQ[s]], semc=sQ[s])
                    for qt in range(4):
                        OP("tensor", [BQ[s], Bkm], [Bps[0]], lambda e, qt=qt, s=s, kmT=kmT: e.matmul(ps[0][:, qt * 32:(qt + 1) * 32], QT[s][:, qt * 128:(qt + 1) * 128], kmT[:], start=True, stop=True))
                    g0 = qc * 4
                    OP("vector", [Bps[0], Bc], [Bgm], lambda e, g0=g0: e.tensor_tensor(gm[:], ps[0][:, 0:128].rearrange("p (a b) -> p a b", b=32), CB[:, g0:g0 + 4, :], ALU.add))
                    for qt in range(4):
                        OP("vector", [Bgm], [Bm8], lambda e, qt=qt: e.max(m8[:, qt, :], gm[:, qt, :]))
                    for qt in range(4):
                        OP("vector", [Bgm, Bm8, Bc], [Bsel], lambda e, qt=qt, g0=g0: e.scalar_tensor_tensor(sel[:, qt, :], gm[:, qt, :], m8[:, qt, 2:3], PAST[:, g0 + qt, :], ALU.is_ge, ALU.mult))
                    OP("vector", [Bsel, Bc], [Bsel], lambda e, g0=g0: e.tensor_tensor(sel[:], sel[:], OWN[:, g0:g0 + 4, :], ALU.add))
                    OP("vector", [Bsel, Brb], [BNM], lambda e, c1=c1: e.tensor_scalar(NM[:], sel[:], c1[:, 0:1], NEGM, ALU.mult, ALU.add))
                    for qt in range(4):
                        OP("tensor", [BNM, Bc], [Bps[1]], lambda e, qt=qt: e.transpose(ps[1][0:32, qt * 128:(qt + 1) * 128], NM[:, qt, :], ident[:]))
                    OP("scalar", [Bps[1]], [BNMT], lambda e: e.activation(NMT[:], ps[1][0:32, :], AF.Copy))
                    nkt = 4 * qc + 4

                    def emit_S(kt, qc=qc, s=s, KT=KT, BKV=BKV, Wn=Wn, BW=BW):
                        bk = 2 + (kt % 2)
                        near = kt >= 4 * qc - 1
                        OP("tensor", [BKV, BQ[s]], [Bps[bk]], lambda e: e.matmul(ps[bk][:], KT[:, kt * 128:(kt + 1) * 128], QT[s][:], start=True, stop=False))
                        OP("tensor", [Bc, BNMT], [Bps[bk]], lambda e: e.matmul(ps[bk][:], Eall[:, kt // 2, :], NMT[:], start=False, stop=(not near)))
                        if near:
                            u0 = 512 * qc - 128 * kt + 384
                            OP("tensor", [Bc, BW], [Bps[bk]], lambda e: e.matmul(ps[bk][:], identb[:], Wn[:, u0:u0 + 512], start=False, stop=True))

                    emit_S(0)
                    for kt in range(nkt):
                        if kt + 1 < nkt:
                            emit_S(kt + 1)
                        bk = 2 + (kt % 2)
                        pslot = pscnt % 3; pscnt += 1
                        OP("scalar", [Bps[bk]], [BPT[pslot]], lambda e, bk=bk, pslot=pslot: e.activation(PT[pslot][:], ps[bk][:], AF.Exp, scale=SC))
                        OP("tensor", [BKV, BPT[pslot]], [Bps[4]], lambda e, kt=kt, pslot=pslot, nkt=nkt, Vs=Vs: e.matmul(ps[4][:], Vs[:, kt, :], PT[pslot][:], start=(kt == 0), stop=(kt == nkt - 1)))
                        OP("tensor", [Bc, BPT[pslot]], [Bps[5]], lambda e, kt=kt, pslot=pslot, nkt=nkt: e.matmul(ps[5][:], ones_b[:], PT[pslot][:], start=(kt == 0), stop=(kt == nkt - 1)))
                    OP("vector", [Bps[5]], [Brinv], lambda e: e.reciprocal(rinv[:], ps[5][:]))
                    OP("vector", [Bps[4], Brinv], [Bob[s]], lambda e, s=s: e.tensor_tensor(ob[s][:], ps[4][:], rinv[:], ALU.mult))
                    P.dma("sync", lambda e, hh=hh, qc=qc, s=s: e.dma_start(out=AGIv[hh][:, qc // 2, (qc % 2) * 512:(qc % 2) * 512 + 512], in_=ob[s][:]),
                          reads=[Bob[s]], writes=[dB["AGI%d" % hh]], semc=sob[s])
            P.replay()

        allgather(1)
        P.replay()

        with ExitStack() as ph:
            def sb(name, shape, dt):
                return ph.enter_context(nc.sbuf_tensor("b1_" + name, list(shape), dt))
            mixT = sb("mixT", [128, 32, TC], BF16); Bmix = Buf("mix")
            gi = sb("gi", [128, 32], I32); Bgi = Buf("gi")
            g2_sb = sb("g2", [128, 32], F32)
            ones_b = sb("ones", [128, 128], BF16)
            P.dma("sync", lambda e: e.dma_start(out=gi[:], in_=gidx[:, :]), writes=[Bgi], semc=None)
            P.dma("sync", lambda e: e.dma_start(out=g2_sb[:], in_=g2[:, :]), writes=[Bgi], semc=None)
            OP("vector", [], [Bgi], lambda e: e.memset(ones_b[:], 1.0))
            sg_ = P.new_sem("gath")
            for ft in range(32):
                kq = (0, 1, 2, 2)[ft % 4]
                P.dma("gpsimd", lambda e, ft=ft, kq=kq: e.indirect_dma_start(out=mixT[:, ft, :], out_offset=None, in_=AGO[kq][:, :],
                                                                      in_offset=bass.IndirectOffsetOnAxis(ap=gi[:, ft:ft + 1], axis=0)),
                      reads=[dB["AGO" + ("0", "1", "r")[kq]], Bgi], writes=[Bmix], semc=sg_)
            if DEBUG:
                P.dma("sync", lambda e: e.dma_start(out=DBGM[:, :, :], in_=mixT[:]), reads=[Bmix], semc=None)
            wo = [sb("wo%d" % i, [128, 32 * 128], BF16) for i in range(2)]; Bwo = [Buf("wo0"), Buf("wo1")]; swo = [P.new_sem("wo0"), P.new_sem("wo1")]
            xr = [sb("xr%d" % i, [128, TC], F32) for i in range(2)]; Bxr = [Buf("xr0"), Buf("xr1")]; sxr = [P.new_sem("xr0"), P.new_sem("xr1")]
            h2 = [sb("h2%d" % i, [128, TC], F32) for i in range(2)]; Bh2 = [Buf("h20"), Buf("h21")]; sh2 = [P.new_sem("h20"), P.new_sem("h21")]
            h2b = sb("h2b", [128, 32, TC], BF16); Bh2b = Buf("h2b")
            sq = [sb("sq%d" % i, [128, TC], BF16) for i in range(2)]; Bsq = [Buf("sq0"), Buf("sq1")]
            for dmt in range(32):
                s = dmt % 2
                P.dma("gpsimd", lambda e, dmt=dmt, s=s: e.dma_start(out=wo[s][:].rearrange("p (a b) -> p a b", b=2048), in_=wod[dmt].rearrange("p f j -> p (f j)").rearrange("p (a b) -> p a b", b=2048)),
                      writes=[Bwo[s]], semc=swo[s])
                P.dma("sync", lambda e, dmt=dmt, s=s: e.dma_start(out=xr[s][:], in_=xTc[dmt * 128:(dmt + 1) * 128, :]), writes=[Bxr[s]], semc=sxr[s])
                for tch in range(2):
                    bk = tch
                    for ft in range(32):
                        OP("tensor", [Bwo[s], Bmix], [Bps[bk]], lambda e, ft=ft, tch=tch, bk=bk, s=s: e.matmul(ps[bk][:], wo[s][:, ft * 128:(ft + 1) * 128], mixT[:, ft, tch * 512:(tch + 1) * 512], start=(ft == 0), stop=(ft == 31)))
                    OP("vector", [Bps[bk], Bxr[s]], [Bh2[s]], lambda e, tch=tch, bk=bk, s=s: e.tensor_tensor(h2[s][:, tch * 512:(tch + 1) * 512], ps[bk][:], xr[s][:, tch * 512:(tch + 1) * 512], ALU.add))
                OP("scalar", [Bh2[s]], [Bsq[s]], lambda e, s=s: e.activation(sq[s][:], h2[s][:], AF.Square))
                OP("scalar", [Bh2[s]], [Bh2b], lambda e, s=s, dmt=dmt: e.activation(h2b[:, dmt, :], h2[s][:], AF.Copy))
                for tch in range(2):
                    OP("tensor", [Bsq[s], Bgi], [Bps[2 + tch]], lambda e, tch=tch, s=s, dmt=dmt: e.matmul(ps[2 + tch][:], ones_b[:], sq[s][:, tch * 512:(tch + 1) * 512], start=(dmt == 0), stop=(dmt == 31)))
                P.dma("sync", lambda e, dmt=dmt, s=s: e.dma_start(out=H2T[dmt * 128:(dmt + 1) * 128, :], in_=h2[s][:]), reads=[Bh2[s]], writes=[dB["H2T"]], semc=sh2[s])
            rs2 = sb("rs2", [128, TC], F32); rs2b = sb("rs2b", [128, TC], F32); Brs2 = Buf("rs2")
            for tch in range(2):
                OP("vector", [Bps[2 + tch]], [Brs2], lambda e, tch=tch: e.tensor_scalar(rs2[:, tch * 512:(tch + 1) * 512], ps[2 + tch][:], 1.0 / D, 1e-6, ALU.mult, ALU.add))
            OP("scalar", [Brs2], [Brs2], lambda e: e.activation(rs2b[:], rs2[:], AF.Sqrt))
            OP("vector", [Brs2], [Brs2], lambda e: e.reciprocal(rs2[:], rs2b[:]))
            xn = [sb("xn%d" % i, [128, TC], BF16) for i in range(2)]; Bxn = [Buf("xn0"), Buf("xn1")]; sxn = [P.new_sem("xn0"), P.new_sem("xn1")]
            for dmt in range(32):
                s = dmt % 2
                OP("vector", [Bh2b, Brs2, Bgi], [Bxn[s]], lambda e, dmt=dmt, s=s: e.scalar_tensor_tensor(xn[s][:], h2b[:, dmt, :], g2_sb[:, dmt:dmt + 1], rs2[:], ALU.mult, ALU.mult))
                P.dma("sync", lambda e, dmt=dmt, s=s: e.dma_start(out=XN2T[dmt * 128:(dmt + 1) * 128, :], in_=xn[s][:]), reads=[Bxn[s]], writes=[dB["XN2T"]], semc=sxn[s])
            P.replay()

        XN2Tv = XN2T.rearrange("(k p) t -> p k t", p=128)

        with ExitStack() as keep:
            def sbk(name, shape, dt):
                return keep.enter_context(nc.sbuf_tensor("pk_" + name, list(shape), dt))
            IDX1T = sbk("IDX1T", [128, TC], F32); IDX2T = sbk("IDX2T", [128, TC], F32); GATET = sbk("GATET", [128, TC], F32)
            BIG = Buf("IG")
            with ExitStack() as ph:
                def sb(name, shape, dt):
                    return ph.enter_context(nc.sbuf_tensor("b2_" + name, list(shape), dt))
                Bc = Buf("c5")
                ident = sb("ident", [128, 128], F32); iota = sb("iota", [128, 128], F32); keyT = sb("keyT", [128, 16, 128], BF16)
                P.dma("sync", lambda e: e.dma_start(out=ident[:], in_=identd[:, :]), writes=[Bc], semc=None)
                P.dma("sync", lambda e: e.dma_start(out=iota[:], in_=iotad[:, :]), writes=[Bc], semc=None)
                P.dma("gpsimd", lambda e: e.dma_start(out=keyT[:], in_=keyTd[:, :, :]), writes=[Bc], semc=None)
                xn2 = sb("xn2", [128, 32, TC], BF16); Bxn2 = Buf("xn2")
                for q4 in range(4):
                    P.dma("sync", lambda e, q4=q4: e.dma_start(out=xn2[:, q4 * 8:(q4 + 1) * 8, :], in_=XN2Tv[:, q4 * 8:(q4 + 1) * 8, :]), reads=[dB["XN2T"]], writes=[Bxn2], semc=None)
                wq = [sb("wq%d" % i, [128, 32 * 128], BF16) for i in range(2)]; Bwq = [Buf("wq0"), Buf("wq1")]; swq = [P.new_sem("wq0"), P.new_sem("wq1")]
                qT = sb("qT", [128, 16, TC], BF16); BqT = Buf("qT")
                for hc in range(16):
                    s = hc % 2
                    P.dma("gpsimd", lambda e, hc=hc, s=s: e.dma_start(out=wq[s][:].rearrange("p (a b) -> p a b", b=2048), in_=wqd[hc].rearrange("p f j -> p (f j)").rearrange("p (a b) -> p a b", b=2048)),
                          writes=[Bwq[s]], semc=swq[s])
                    for tch in range(2):
                        bk = tch
                        for kt in range(32):
                            OP("tensor", [Bwq[s], Bxn2], [Bps[bk]], lambda e, kt=kt, tch=tch, bk=bk, s=s: e.matmul(ps[bk][:], wq[s][:, kt * 128:(kt + 1) * 128], xn2[:, kt, tch * 512:(tch + 1) * 512], start=(kt == 0), stop=(kt == 31)))
                        OP("scalar", [Bps[bk]], [BqT], lambda e, hc=hc, tch=tch, bk=bk: e.activation(qT[:, hc, tch * 512:(tch + 1) * 512], ps[bk][:], AF.Copy))
                ssb = sb("ssb", [128, 16, 128], F32); Bss = Buf("ss")
                Bssl = [Buf("ss%d" % i) for i in range(16)]; Bsvl = [Buf("sv%d" % i) for i in range(16)]; Bsil = [Buf("si%d" % i) for i in range(16)]
                Bcl = [Buf("c%d" % i) for i in range(8)]; Btl = [Buf("t%d" % i) for i in range(8)]; Bpl = [Buf("p%d" % i) for i in range(8)]
                sv = sb("sv", [128, 16, 16], F32); siu = sb("siu", [128, 16, 16], U32); sif = sb("sif", [128, 16, 16], F32); Bsv = Buf("sv")
                cand = sb("cand", [128, 8, 256], F32); Bcand = Buf("cand")
                tops = sb("tops", [128, 8, 16], F32); posu = sb("posu", [128, 8, 16], U32); posf = sb("posf", [128, 8, 16], F32); Btop = Buf("top")
                af = sb("af", [128, 8, 16], F32); bf = sb("bf", [128, 8, 16], F32); au = sb("au", [128, 8, 16], U32); bu = sb("bu", [128, 8, 16], U32)
                oh = sb("oh", [128, 8, 16, 16], F32); Boh = Buf("oh")
                idx1 = sb("idx1", [128, 128], F32); idx2 = sb("idx2", [128, 128], F32); gate = sb("gate", [128, 128], F32); Big = Buf("ig")
                zz = sb("zz", [128, 8], F32)
                for tt in range(8):
                    for hc in range(16):
                        bk = 2 + hc // 4
                        OP("tensor", [BqT, Bc], [Bps[bk]], lambda e, hc=hc, tt=tt, bk=bk: e.matmul(ps[bk][:, (hc % 4) * 128:(hc % 4 + 1) * 128], qT[:, hc, tt * 128:(tt + 1) * 128], keyT[:, hc, :], start=True, stop=True))
                    for g in range(4):
                        OP("scalar", [Bps[2 + g]], Bssl[g * 4:(g + 1) * 4], lambda e, g=g: e.activation(ssb[:, g * 4:(g + 1) * 4, :], ps[2 + g][:].rearrange("p (a b) -> p a b", b=128), AF.Copy))
                    for hc in range(16):
                        OP("vector", [Bssl[hc]], [Bsvl[hc]], lambda e, hc=hc: e.max(sv[:, hc, 0:8], ssb[:, hc, :]))
                    for hc in range(16):
                        OP("vector", [Bssl[hc], Bsvl[hc]], [Bsil[hc]], lambda e, hc=hc: e.max_index(siu[:, hc, 0:8], sv[:, hc, 0:8], ssb[:, hc, :]))
                    for hc in range(16):
                        OP("vector", [Bssl[hc], Bsvl[hc]], [Bssl[hc]], lambda e, hc=hc: e.match_replace(ssb[:, hc, :], sv[:, hc, 0:8], ssb[:, hc, :], -1e30))
                    for hc in range(16):
                        OP("vector", [Bssl[hc]], [Bsvl[hc]], lambda e, hc=hc: e.max(sv[:, hc, 8:16], ssb[:, hc, :]))
                    for hc in range(16):
                        OP("vector", [Bssl[hc], Bsvl[hc]], [Bsil[hc]], lambda e, hc=hc: e.max_index(siu[:, hc, 8:16], sv[:, hc, 8:16], ssb[:, hc, :]))
                    OP("vector", Bsil, [Bsv], lambda e: e.tensor_copy(sif[:], siu[:]))
                    svv = sv[:].rearrange("p (h c) k -> p h c k", c=2)
                    sifv = sif[:].rearrange("p (h c) k -> p h c k", c=2)
                    for h in range(8):
                        OP("vector", [Bsvl[2 * h], Bsvl[2 * h + 1]], [Bcl[h]], lambda e, h=h, svv=svv: e.tensor_tensor(cand[:, h, :].rearrange("p (a b) -> p a b", b=16),
                                                                                 svv[:, h, 0, :].unsqueeze(2).broadcast_to([128, 16, 16]),
                                                                                 svv[:, h, 1, :].unsqueeze(1).broadcast_to([128, 16, 16]), ALU.add))
                    for h in range(8):
                        OP("vector", [Bcl[h]], [Btl[h]], lambda e, h=h: e.max(tops[:, h, 0:8], cand[:, h, :]))
                    for h in range(8):
                        OP("vector", [Bcl[h], Btl[h]], [Bpl[h]], lambda e, h=h: e.max_index(posu[:, h, 0:8], tops[:, h, 0:8], cand[:, h, :]))
                    for h in range(8):
                        OP("vector", [Bcl[h], Btl[h]], [Bcl[h]], lambda e, h=h: e.match_replace(cand[:, h, :], tops[:, h, 0:8], cand[:, h, :], -1e30))
                    for h in range(8):
                        OP("vector", [Bcl[h]], [Btl[h]], lambda e, h=h: e.max(tops[:, h, 8:16], cand[:, h, :]))
                    for h in range(8):
                        OP("vector", [Bcl[h], Btl[h]], [Bpl[h]], lambda e, h=h: e.max_index(posu[:, h, 8:16], tops[:, h, 8:16], cand[:, h, :]))
                    OP("vector", Bpl + Btl, [Btop], lambda e: e.tensor_scalar(au[:], posu[:], 4, None, ALU.logical_shift_right))
                    OP("vector", [Btop], [Btop], lambda e: e.tensor_scalar(bu[:], posu[:], 15, None, ALU.bitwise_and))
                    OP("vector", [Btop], [Btop], lambda e: e.tensor_copy(af[:], au[:]))
                    OP("vector", [Btop], [Btop], lambda e: e.tensor_copy(bf[:], bu[:]))
                    for (sel_, c, dst) in ((af, 0, idx1), (bf, 1, idx2)):
                        for h in range(8):
                            OP("vector", [Btop, Bc], [Boh], lambda e, h=h, sel_=sel_: e.tensor_tensor(oh[:, h, :, :], iota[:, 0:16].unsqueeze(1).broadcast_to([128, 16, 16]),
                                                                                         sel_[:, h, :].unsqueeze(2).broadcast_to([128, 16, 16]), ALU.is_equal))
                            OP("vector", [Boh, Bsv], [Boh], lambda e, h=h, c=c, sifv=sifv: e.tensor_tensor(oh[:, h, :, :], oh[:, h, :, :], sifv[:, h, c, :].unsqueeze(1).broadcast_to([128, 16, 16]), ALU.mult))
                        OP("vector", [Boh], [Big], lambda e, dst=dst: e.tensor_reduce(dst[:], oh[:].rearrange("p h k a -> p (h k) a"), AX.X, ALU.add))
                    OP("vector", [Btop] + Btl, [Btop], lambda e: e.tensor_tensor(posf[:], tops[:], tops[:, :, 0:1].broadcast_to([128, 8, 16]), ALU.subtract))
                    OP("scalar", [Btop], [Btop], lambda e: e.activation(posf[:], posf[:], AF.Exp))
                    OP("vector", [Btop], [Btop], lambda e: e.tensor_reduce(zz[:], posf[:], AX.X, ALU.add))
                    OP("vector", [Btop], [Btop], lambda e: e.reciprocal(zz[:], zz[:]))
                    OP("vector", [Btop], [Big], lambda e: e.tensor_tensor(gate[:].rearrange("p (h k) -> p h k", k=16), posf[:], zz[:].unsqueeze(2).broadcast_to([128, 8, 16]), ALU.mult))
                    for j, (src, dstT) in enumerate(((idx1, IDX1T), (idx2, IDX2T), (gate, GATET))):
                        bk = 6 + (j % 2)
                        OP("tensor", [Big, Bc], [Bps[bk]], lambda e, src=src, bk=bk: e.transpose(ps[bk][:, 0:128], src[:], ident[:]))
                        OP("vector", [Bps[bk]], [BIG], lambda e, dstT=dstT, tt=tt, bk=bk: e.tensor_copy(dstT[:, tt * 128:(tt + 1) * 128], ps[bk][:, 0:128]))
                P.replay()

            Gd = dscr("Gd", [128, 128, TC], BF16); Wd = dscr("Wd", [128, 128, TC], BF16)
            BGd = Buf("Gd"); BWd = Buf("Wd")
            Gdv = Gd.rearrange("i j t -> j i t")
            with ExitStack() as ph:
                def sb(name, shape, dt):
                    return ph.enter_context(nc.sbuf_tensor("b3a_" + name, list(shape), dt))
                Bc = Buf("c6")
                iotaf = sb("iotaf", [128, 128], F32); iotab = sb("iotab", [128, 128], BF16)
                P.dma("sync", lambda e: e.dma_start(out=iotaf[:], in_=iotad[:, :]), writes=[Bc], semc=None)
                OP("vector", [Bc], [Bc], lambda e: e.tensor_copy(iotab[:], iotaf[:]))
                TT = 256
                NSB = 32
                Gst = [sb("Gst%d" % i, [128, 128, TT], BF16) for i in range(2)]; BGst = [Buf("g0"), Buf("g1")]; sGst = [P.new_sem("g0"), P.new_sem("g1")]
                Lr = [sb("L%d" % i, [128, NSB, 128], BF16) for i in range(2)]; Rr = [sb("R%d" % i, [128, NSB, 128], BF16) for i in range(2)]
                BL = [Buf("l0"), Buf("l1")]; BR = [Buf("r0"), Buf("r1")]
                gcnt = 0
                for T in range(TC // TT):
                    tb = T * TT
                    gs = T % 2
                    for sbk_ in range(TT // NSB):
                        s = sbk_ % 2
                        c0 = tb + sbk_ * NSB
                        for tl in range(NSB):
                            OP("vector", [Bc, BIG], [BL[s]], lambda e, s=s, tl=tl, c0=c0: e.tensor_scalar(Lr[s][:, tl, :], iotab[:], IDX1T[:, c0 + tl:c0 + tl + 1], GATET[:, c0 + tl:c0 + tl + 1], ALU.is_equal, ALU.mult))
                            OP("vector", [Bc, BIG], [BR[s]], lambda e, s=s, tl=tl, c0=c0: e.tensor_scalar(Rr[s][:, tl, :], iotab[:], IDX2T[:, c0 + tl:c0 + tl + 1], None, ALU.is_equal))
                        for g16 in range(NSB // 16):
                            half = gcnt % 2; gcnt += 1
                            pst = psA if half == 0 else psB
                            for k in range(16):
                                tl = g16 * 16 + k
                                OP("tensor", [BL[s], BR[s]], [Bps[half * 4 + k // 4]], lambda e, s=s, tl=tl, k=k, pst=pst: e.matmul(pst[:, k * 128:(k + 1) * 128], Rr[s][:, tl, :], Lr[s][:, tl, :], start=True, stop=True))
                            tloc = sbk_ * NSB + g16 * 16
                            OP("scalar", [Bps[half * 4 + q] for q in range(4)], [BGst[gs]], lambda e, pst=pst, tloc=tloc, gs=gs: e.activation(Gst[gs][:, :, tloc:tloc + 16], pst[:, :].rearrange("p (t i) -> p i t", i=128), AF.Copy))
                    for i8 in range(8):
                        P.dma("sync", lambda e, i8=i8, gs=gs, tb=tb: e.dma_start(out=Gdv[:, i8 * 16:(i8 + 1) * 16, tb:tb + TT], in_=Gst[gs][:, i8 * 16:(i8 + 1) * 16, :]),
                              reads=[BGst[gs]], writes=[BGd], semc=sGst[gs])
                P.replay()
            with ExitStack() as ph:
                def sb(name, shape, dt):
                    return ph.enter_context(nc.sbuf_tensor("b3b_" + name, list(shape), dt))
                xn2 = sb("xn2", [128, 32, TC], BF16); Bxn2 = Buf("xn2")
                for q4 in range(4):
                    P.dma("sync", lambda e, q4=q4: e.dma_start(out=xn2[:, q4 * 8:(q4 + 1) * 8, :], in_=XN2Tv[:, q4 * 8:(q4 + 1) * 8, :]), reads=[dB["XN2T"]], writes=[Bxn2], semc=None)
                UTs = [sb("UT%d" % i, [128, 32 * 128], BF16) for i in range(3)]; BUT = [Buf("u%d" % i) for i in range(3)]; sUT = [P.new_sem("u%d" % i) for i in range(3)]
                Gi = [sb("Gi%d" % i, [128, TC], BF16) for i in range(3)]; BGi = [Buf("gi%d" % i) for i in range(3)]; sGi = [P.new_sem("gi%d" % i) for i in range(3)]
                ag = [sb("ag%d" % i, [128, TC], F32) for i in range(2)]; Bag = [Buf("ag0"), Buf("ag1")]
                Wst = [sb("Wst%d" % i, [128, TC], BF16) for i in range(2)]; BWst = [Buf("w0"), Buf("w1")]; sWst = [P.new_sem("w0"), P.new_sem("w1")]
                for i in range(128):
                    s3 = i % 3; s = i % 2
                    P.dma("gpsimd", lambda e, i=i, s3=s3: e.dma_start(out=UTs[s3][:].rearrange("p (a b) -> p a b", b=2048), in_=UTd[i].rearrange("p (a b) -> p a b", b=2048)), writes=[BUT[s3]], semc=sUT[s3])
                    P.dma("sync", lambda e, i=i, s3=s3: e.dma_start(out=Gi[s3][:], in_=Gd[i, :, :]), reads=[BGd], writes=[BGi[s3]], semc=sGi[s3])
                    for tch in range(2):
                        bk = (i % 2) * 2 + tch
                        for kt in range(32):
                            OP("tensor", [BUT[s3], Bxn2], [Bps[bk]], lambda e, kt=kt, bk=bk, s3=s3, tch=tch: e.matmul(ps[bk][:], UTs[s3][:, kt * 128:(kt + 1) * 128], xn2[:, kt, tch * 512:(tch + 1) * 512], start=(kt == 0), stop=(kt == 31)))
                        OP("scalar", [Bps[bk]], [Bag[s]], lambda e, bk=bk, s=s, tch=tch: e.activation(ag[s][:, tch * 512:(tch + 1) * 512], ps[bk][:], AF.Gelu))
                    OP("vector", [Bag[s], BGi[s3]], [BWst[s]], lambda e, s=s, s3=s3: e.tensor_tensor(Wst[s][:], ag[s][:], Gi[s3][:], ALU.mult))
                    P.dma("sync", lambda e, i=i, s=s: e.dma_start(out=Wd[i, :, :], in_=Wst[s][:]), reads=[BWst[s]], writes=[BWd], semc=sWst[s])
                P.replay()
            with ExitStack() as ph:
                def sb(name, shape, dt):
                    return ph.enter_context(nc.sbuf_tensor("b3c_" + name, list(shape), dt))
                Wi = [sb("Wi%d" % i, [128, TC], BF16) for i in range(4)]; BWi = [Buf("wi%d" % i) for i in range(4)]; sWi = [P.new_sem("wi%d" % i) for i in range(4)]
                Vh = [sb("Vh%d" % i, [128, 512], BF16) for i in range(4)]; BVh = [Buf("v%d" % i) for i in range(4)]; sVh = [P.new_sem("v%d" % i) for i in range(4)]
                h2r = sb("h2r", [128, 4, TC], F32); Bh2r = Buf("h2r"); sh2r = P.new_sem("h2r")
                h3 = sb("h3", [128, 4, TC], F32); Bh3 = Buf("h3"); sh3 = P.new_sem("h3")
                H2Tv = H2T.rearrange("(k p) t -> p k t", p=128); H3Tv = H3T.rearrange("(k p) t -> p k t", p=128)
                cnt = 0
                NRES = 64
                Wres = sb("Wres", [128, NRES, TC], BF16); BWres = Buf("Wres"); sWres = P.new_sem("wres")
                Wdv = Wd.rearrange("i j t -> j i t")
                for r8 in range(NRES // 8):
                    P.dma("sync", lambda e, r8=r8: e.dma_start(out=Wres[:, r8 * 8:(r8 + 1) * 8, :], in_=Wdv[:, r8 * 8:(r8 + 1) * 8, :]), reads=[BWd], writes=[BWres], semc=sWres)
                for sw in range(8):
                    P.dma("sync", lambda e, sw=sw: e.dma_start(out=h2r[:], in_=H2Tv[:, sw * 4:(sw + 1) * 4, :]), reads=[dB["H2T"]], writes=[Bh2r], semc=sh2r)
                    for i in range(128):
                        s = cnt % 4; cnt += 1
                        if i >= NRES:
                            P.dma("sync", lambda e, i=i, s=s: e.dma_start(out=Wi[s][:], in_=Wd[i, :, :]), reads=[BWd], writes=[BWi[s]], semc=sWi[s])
                        P.dma("gpsimd", lambda e, i=i, s=s, sw=sw: e.dma_start(out=Vh[s][:], in_=Vd[i * 128:(i + 1) * 128, sw * 512:(sw + 1) * 512]), writes=[BVh[s]], semc=sVh[s])
                        for dmt in range(4):
                            for tch in range(2):
                                bk = dmt * 2 + tch
                                if i < NRES:
                                    OP("tensor", [BVh[s], BWres], [Bps[bk]], lambda e, dmt=dmt, tch=tch, s=s, bk=bk, i=i: e.matmul(ps[bk][:], Vh[s][:, dmt * 128:(dmt + 1) * 128], Wres[:, i, tch * 512:(tch + 1) * 512], start=(i == 0), stop=(i == 127)))
                                else:
                                    OP("tensor", [BVh[s], BWi[s]], [Bps[bk]], lambda e, dmt=dmt, tch=tch, s=s, bk=bk, i=i: e.matmul(ps[bk][:], Vh[s][:, dmt * 128:(dmt + 1) * 128], Wi[s][:, tch * 512:(tch + 1) * 512], start=(i == 0), stop=(i == 127)))
                    for dmt in range(4):
                        for tch in range(2):
                            bk = dmt * 2 + tch
                            OP("vector", [Bps[bk], Bh2r], [Bh3], lambda e, bk=bk, dmt=dmt, tch=tch: e.tensor_tensor(h3[:, dmt, tch * 512:(tch + 1) * 512], ps[bk][:], h2r[:, dmt, tch * 512:(tch + 1) * 512], ALU.add))
                    P.dma("sync", lambda e, sw=sw: e.dma_start(out=H3Tv[:, sw * 4:(sw + 1) * 4, :], in_=h3[:]), reads=[Bh3], writes=[dB["H3T"]], semc=sh3)
                P.replay()

        with ExitStack() as ph:
            def sb(name, shape, dt):
                return ph.enter_context(nc.sbuf_tensor("b4_" + name, list(shape), dt))
            Bc = Buf("c7")
            gf_sb = sb("gf", [128, 32], F32); ones_b = sb("ones", [128, 128], BF16)
            P.dma("sync", lambda e: e.dma_start(out=gf_sb[:], in_=gf[:, :]), writes=[Bc], semc=None)
            OP("vector", [], [Bc], lambda e: e.memset(ones_b[:], 1.0))
            hh3 = [sb("h%d" % i, [128, 32, 256], F32) for i in range(2)]; Bhh = [Buf("h0"), Buf("h1")]; shh = [P.new_sem("h0"), P.new_sem("h1")]
            sqq = sb("sqq", [128, 32, 256], BF16); Bsqq = Buf("sqq")
            r1 = sb("r1", [128, 256], F32); r2 = sb("r2", [128, 256], F32); Br = Buf("r")
            yo = [sb("yo%d" % i, [128, 8, 256], F32) for i in range(2)]; Byo = [Buf("yo0"), Buf("yo1")]; syo = [P.new_sem("yo0"), P.new_sem("yo1")]
            H3Tv = H3T.rearrange("(k p) t -> p k t", p=128); YTv = YT.rearrange("(k p) t -> p k t", p=128)
            yc = 0
            for T in range(4):
                s = T % 2
                tb = T * 256
                P.dma("sync", lambda e, s=s, tb=tb: e.dma_start(out=hh3[s][:], in_=H3Tv[:, :, tb:tb + 256]), reads=[dB["H3T"]], writes=[Bhh[s]], semc=shh[s])
                OP("scalar", [Bhh[s]], [Bsqq], lambda e, s=s: e.activation(sqq[:], hh3[s][:], AF.Square))
                for kt in range(32):
                    OP("tensor", [Bsqq, Bc], [Bps[0]], lambda e, kt=kt: e.matmul(ps[0][:, 0:256], ones_b[:], sqq[:, kt, :], start=(kt == 0), stop=(kt == 31)))
                OP("vector", [Bps[0]], [Br], lambda e: e.tensor_scalar(r1[:], ps[0][:, 0:256], 1.0 / D, 1e-6, ALU.mult, ALU.add))
                OP("scalar", [Br], [Br], lambda e: e.activation(r2[:], r1[:], AF.Sqrt))
                OP("vector", [Br], [Br], lambda e: e.reciprocal(r1[:], r2[:]))
                for q in range(4):
                    ys = yc % 2; yc += 1
                    for k in range(8):
                        kt = q * 8 + k
                        OP("vector", [Bhh[s], Br, Bc], [Byo[ys]], lambda e, kt=kt, k=k, s=s, ys=ys: e.scalar_tensor_tensor(yo[ys][:, k, :], hh3[s][:, kt, :], gf_sb[:, kt:kt + 1], r1[:], ALU.mult, ALU.mult))
                    P.dma("sync", lambda e, q=q, ys=ys, tb=tb: e.dma_start(out=YTv[:, q * 8:(q + 1) * 8, tb:tb + 256], in_=yo[ys][:]), reads=[Byo[ys]], semc=syo[ys])
            P.replay()
    return nc


def _t5_bucket(n):
    n = np.maximum(n, 0)
    nf = np.maximum(n, 1).astype(np.float32)
    large = 16 + (np.log(nf / np.float32(16)) / np.float32(math.log(128 / 16)) * np.float32(16)).astype(np.int32)
    large = np.minimum(large, 31)
    return np.where(n < 16, n, large)


def _host_consts():
    c = {}
    half = 128
    inv = (np.float32(10000.0) ** (-np.arange(half, dtype=np.float32) / np.float32(half))).astype(np.float32)
    ang = np.arange(S, dtype=np.float32)[:, None] * inv[None, :]
    cos = np.cos(ang).astype(np.float32); sin = np.sin(ang).astype(np.float32)
    c["cosT"] = np.ascontiguousarray(cos.T); c["sinT"] = np.ascontiguousarray(sin.T)
    qt = np.arange(64)[:, None]; blk = np.arange(32)[None, :]
    cur = qt // 2
    CB = np.where(blk < cur, 0.0, -1e30).astype(np.float32)
    PAST = (blk < cur).astype(np.float32)
    OWN = (blk == cur).astype(np.float32)
    for nm, a in (("CB", CB), ("PAST", PAST), ("OWN", OWN)):
        c[nm] = np.ascontiguousarray(np.broadcast_to(a[None], (128, 64, 32))).astype(np.float32)
    E = np.zeros((32, 32, 128), np.float32)
    for b in range(32):
        E[b, b, :] = 1.0
    c["Eall"] = E.astype(ml_dtypes.bfloat16)
    k = np.arange(128)[:, None]; u = np.arange(1024)[None, :]
    dist = u - 384 - k
    bucket = _t5_bucket(dist)
    OH = np.zeros((128, 31, 1024), np.float32)
    for b in range(31):
        OH[:, b, :] = ((bucket == b) & (dist >= 0))
    c["OH"] = OH.astype(ml_dtypes.bfloat16)
    c["CAUSW"] = np.where(dist >= 0, 0.0, NEGM).astype(np.float32)
    c["identf"] = np.eye(128, dtype=np.float32)
    c["iota128"] = np.ascontiguousarray(np.broadcast_to(np.arange(128, dtype=np.float32)[None], (128, 128)))
    return c


def _ret_consts(h):
    g = np.float64(1.0) - np.float64(2.0) ** (-5.0 - h)
    lg = np.log(g)
    j = np.arange(512)
    jj = (np.arange(4)[None, :, None] * 128 + np.arange(128)[:, None, None])
    ii = np.arange(512)[None, None, :]
    diff = ii - jj
    DT = np.where(diff >= 0, np.exp(lg * np.maximum(diff, 0)), 0.0).astype(np.float32)
    QD = np.broadcast_to(np.exp(lg * (j + 1.0))[None, :], (128, 512)).astype(np.float32)
    CD = np.full((128, 1), np.exp(lg * 512.0), np.float32)
    t = np.arange(64)[None, :] * 128 + np.arange(128)[:, None]
    kd = np.exp(lg * (511.0 - (t % 512))).astype(np.float32)
    return np.ascontiguousarray(DT), np.ascontiguousarray(QD), CD, np.ascontiguousarray(kd)


def _tile_w(w):
    C = w.shape[1]
    return np.ascontiguousarray(w.reshape(32, 128, C).transpose(1, 0, 2))


def _vecpk(v):
    return np.ascontiguousarray(v.reshape(32, 128).T.astype(np.float32))


_CACHE = {}


def kernel(x, norm_mix_g, w_in, w_out, rel_bias, norm_ffn_g, peer_w_q, peer_sub_keys, peer_u, peer_v, norm_final_g):
    x = np.asarray(x, np.float32)[0]
    w_in = np.asarray(w_in, np.float32)[0]; w_out = np.asarray(w_out, np.float32)[0]
    rel_bias = np.asarray(rel_bias, np.float32)
    wq = np.asarray(peer_w_q, np.float32)[0]; sk = np.asarray(peer_sub_keys, np.float32)[0]
    u = np.asarray(peer_u, np.float32)[0]; v = np.asarray(peer_v, np.float32)[0]
    if "nc" not in _CACHE:
        _CACHE["nc"] = build_nc()
        _CACHE["c"] = _host_consts()
    nc = _CACHE["nc"]; C = _CACHE["c"]
    xT = np.ascontiguousarray(x.T)
    shared = dict(C)
    shared["xT"] = xT
    shared["g1"] = _vecpk(np.asarray(norm_mix_g)[0]); shared["g2"] = _vecpk(np.asarray(norm_ffn_g)[0]); shared["gf"] = _vecpk(np.asarray(norm_final_g))
    perm = np.concatenate([np.concatenate([256 * r + np.arange(256), 2048 + 256 * r + np.arange(256)]) for r in range(8)])
    wo_p = w_out[perm]
    shared["wo"] = np.ascontiguousarray(wo_p.reshape(32, 128, 32, 128).transpose(2, 1, 0, 3))
    shared["wq"] = np.ascontiguousarray(wq.reshape(32, 128, 16, 128).transpose(2, 1, 0, 3))
    shared["keyT"] = np.ascontiguousarray(sk.reshape(16, 128, 128).transpose(2, 0, 1))
    shared["UT"] = np.ascontiguousarray(u.reshape(128, 128, 32, 128).transpose(0, 3, 2, 1)).reshape(128, 128, 4096)
    shared["V"] = np.ascontiguousarray(v)
    in_maps = []
    for c in range(NCORE):
        m = dict(shared)
        m["xTc"] = np.ascontiguousarray(xT[:, c * TC:(c + 1) * TC])
        h0, h1 = 2 * c, 2 * c + 1
        mq = lambda h: np.arange(h * 128, (h + 1) * 128)
        fm_cols = np.concatenate([mq(h0), mq(h1), 2048 + mq(h0), 2048 + mq(h1),
                                  6144 + c * 256 + np.arange(256), 8192 + c * 256 + np.arange(256), 12288 + c * 256 + np.arange(256)])
        tm_cols = np.concatenate([4096 + mq(h0), 4096 + mq(h1), 10240 + c * 256 + np.arange(256)])
        m["wfm"] = _tile_w(w_in[:, fm_cols]); m["wtm"] = _tile_w(w_in[:, tm_cols])
        DT, QD, CD, kd = _ret_consts(c)
        m["DT"] = DT; m["QD"] = QD; m["CD"] = CD; m["kdec"] = kd
        m["rbc"] = np.ascontiguousarray(rel_bias[:, [h0, h1]].T)
        ftv = np.arange(32)[None, :]; pv = np.arange(128)[:, None]
        rr = ftv // 4; qq = ftv % 4
        gm_ = np.where(qq < 2, rr * 1024 + pv * 8 + c, rr * 2048 + ((qq - 2) * 128 + pv) * 8 + c)
        m["gidx"] = gm_.astype(np.int32)
        in_maps.append(m)
    res = run_bass_kernel_spmd(nc, in_maps, core_ids=list(range(NCORE)))
    _CACHE["res"] = res
    y = np.concatenate([np.asarray(r["YT"]) for r in res.results], axis=1)
    return np.ascontiguousarray(y.T).reshape(1, S, D).astype(np.float32)
```
